# Optimizing a Trainium2 kernel written in Bass

```python
import math
import jax, jax.numpy as jnp
from jax import lax
import numpy as np

D_MODEL = 1024
BATCH = 2
SEQ = 8192
DEPTH = 1

GRID_W = 64
CTX_LEN = 256
EPS = 1e-6
M_HEADS = 4
M_HEAD_DIM = 256
M_WIDTH = M_HEADS * M_HEAD_DIM
M_CHUNK = 64
H_WIDTH = 1024
H_POS_BANDS = 16
H_POS_DIM = 1 + 2 * H_POS_BANDS
H_FILTER_HIDDEN = 64
H_FAST_DECAY_PCT = 0.3
H_SLOW_DECAY_PCT = 1.5
H_DECAY_TARGET = 1e-2
SHORT_CONV = 3
N_GROUPS = 8
EXPERTS_PER_GROUP = 8
N_EXPERTS = N_GROUPS * EXPERTS_PER_GROUP
TOP_K_IN_GROUP = 2
D_EXPERT = 512
MOE_BLOCK = 128
Q0 = 0
K0 = Q0 + M_WIDTH
V0 = K0 + M_WIDTH
O0 = V0 + M_WIDTH
IG0 = O0 + M_WIDTH
FG0 = IG0 + 2 * M_HEADS
M_COLS = FG0 + 2 * M_HEADS
HY0 = M_COLS
GA0 = HY0 + 3 * H_WIDTH
GB0 = GA0 + D_MODEL
IN_COLS = GB0 + D_MODEL

kernel_name = 'hybrid_mlstm_hyena_hmoe_dit_block'


def rmsnorm(x, g):
    xf = x.astype(jnp.float32)
    y = xf * lax.rsqrt(jnp.mean(xf * xf, axis=-1, keepdims=True) + EPS)
    return (y * g.astype(jnp.float32)).astype(x.dtype)


def adaln(cond, w_mod, b_mod):
    m = jax.nn.silu(cond) @ w_mod + b_mod
    return m.reshape(*cond.shape[:-1], 6, D_MODEL)


def modulate(x, g, mod, i):
    return rmsnorm(x, g) * (1 + mod[..., i + 1, :]) + mod[..., i, :]


def short_conv(u, w, b, rows):
    bsz, L, ch = u.shape
    u4 = u.reshape(bsz, rows, L // rows, ch)
    up = jnp.pad(u4, ((0, 0), (0, 0), (1, 1), (0, 0)))
    y = up[:, :, :-2] * w[0] + up[:, :, 1:-1] * w[1] + up[:, :, 2:] * w[2] + b
    return y.reshape(bsz, L, ch)


def mlstm_chunkwise(q, k, v, ig, lf, c0, n0, m0):
    bsz, grp, L, dh = q.shape
    nc = L // M_CHUNK

    def chunks(t):
        return jnp.moveaxis(t.reshape(bsz, grp, nc, M_CHUNK, *t.shape[3:]), 2, 0)

    tril = jnp.tril(jnp.ones((M_CHUNK, M_CHUNK), bool))

    def step(carry, inp):
        cmat, nvec, m = carry
        qc, kc, vc, ic, fc = inp
        b = jnp.cumsum(fc, axis=-1)
        dmat = jnp.where(tril, b[..., :, None] - b[..., None, :] + ic[..., None, :], -jnp.inf)
        inter = b + m[..., None]
        m_t = jnp.maximum(inter, jnp.max(dmat, axis=-1))
        s = jnp.einsum('bgtd,bgsd->bgts', qc, kc) * jnp.exp(dmat - m_t[..., None])
        carry_scale = jnp.exp(inter - m_t)
        num = (jnp.einsum('bgts,bgse->bgte', s, vc)
               + carry_scale[..., None] * jnp.einsum('bged,bgtd->bgte', cmat, qc))
        den = jnp.sum(s, axis=-1) + carry_scale * jnp.einsum('bgd,bgtd->bgt', nvec, qc)
        h = num / jnp.maximum(jnp.abs(den), jnp.exp(-m_t))[..., None]
        b_last = b[..., -1]
        g = b_last[..., None] - b + ic
        m_new = jnp.maximum(b_last + m, jnp.max(g, axis=-1))
        wgt = jnp.exp(g - m_new[..., None])
        decay = jnp.exp(b_last + m - m_new)
        c_new = decay[..., None, None] * cmat + jnp.einsum('bgs,bgse,bgsd->bged', wgt, vc, kc)
        n_new = decay[..., None] * nvec + jnp.einsum('bgs,bgsd->bgd', wgt, kc)
        return (c_new, n_new, m_new), h

    (cf, nf, mf), hs = lax.scan(step, (c0, n0, m0),
                                (chunks(q), chunks(k), chunks(v), chunks(ig), chunks(lf)))
    h = jnp.moveaxis(hs, 0, 2).reshape(bsz, grp, L, dh)
    return h, (cf, nf, mf)


def mlstm_branch(z, w_conv, b_conv, rows, state):
    bsz, L, _ = z.shape
    f32 = jnp.float32
    qk = jax.nn.silu(short_conv(z[..., Q0:V0], w_conv, b_conv, rows))
    q = qk[..., :M_WIDTH]
    k = qk[..., M_WIDTH:] * (M_HEAD_DIM ** -0.5)
    v = z[..., V0:O0]
    o = jax.nn.sigmoid(z[..., O0:IG0])
    ig = z[..., IG0:FG0]
    lf = jax.nn.log_sigmoid(z[..., FG0:M_COLS].astype(f32))

    def heads_dirs(t):
        t = t.astype(f32).reshape(bsz, L, M_HEADS, M_HEAD_DIM)
        t = jnp.stack([t, t[:, ::-1]], axis=1)
        return t.transpose(0, 1, 3, 2, 4).reshape(bsz, 2 * M_HEADS, L, M_HEAD_DIM)

    def gates_dirs(g):
        g = g.astype(f32).reshape(bsz, L, 2, M_HEADS)
        g = jnp.stack([g[:, :, 0], g[:, ::-1, 1]], axis=1)
        return g.transpose(0, 1, 3, 2).reshape(bsz, 2 * M_HEADS, L)

    h, state_out = mlstm_chunkwise(heads_dirs(q), heads_dirs(k), heads_dirs(v),
                                   gates_dirs(ig), gates_dirs(lf), *state)
    h = h.reshape(bsz, 2, M_HEADS, L, M_HEAD_DIM)
    h = h[:, 0] + h[:, 1, :, ::-1]
    h = h.transpose(0, 2, 1, 3).reshape(bsz, L, M_WIDTH)
    return o * h.astype(z.dtype), state_out


def hyena_filter(L, w1, b1, w2, b2, w3, freq):
    f32 = jnp.float32
    pos = jnp.arange(L, dtype=f32)
    t = (pos / max(L - 1, 1))[:, None]
    bands = jnp.linspace(1e-4, H_POS_BANDS - 1, H_POS_BANDS, dtype=f32)
    ang = (2 * math.pi / L) * pos[:, None] * bands[None]
    feats = jnp.concatenate([t, jnp.cos(ang), -jnp.sin(ang)], axis=-1)
    freq = freq.astype(f32)
    hid = jnp.sin(freq * (feats @ w1.astype(f32) + b1.astype(f32)))
    hid = jnp.sin(freq * (hid @ w2.astype(f32) + b2.astype(f32)))
    filt = hid @ w3.astype(f32)
    max_decay = math.log(H_DECAY_TARGET) / H_FAST_DECAY_PCT
    min_decay = math.log(H_DECAY_TARGET) / H_SLOW_DECAY_PCT
    deltas = jnp.linspace(min_decay, max_decay, H_WIDTH, dtype=f32)
    window = jnp.exp(-t * jnp.abs(deltas)[None])
    fwd = filt[:, :H_WIDTH] * window
    bwd = filt[:, H_WIDTH:] * window
    kern = jnp.concatenate([fwd, jnp.zeros((1, H_WIDTH), f32), bwd[:0:-1]], axis=0)
    return kern * lax.rsqrt(jnp.sum(kern * kern, axis=0, keepdims=True) + EPS)


def hyena_branch(z, w_conv, b_conv, f_w1, f_b1, f_w2, f_b2, f_w3, f_freq, h_bias, rows):
    bsz, L, _ = z.shape
    u = short_conv(z[..., HY0:GA0], w_conv, b_conv, rows)
    x0 = u[..., :H_WIDTH]
    x1 = u[..., H_WIDTH:2 * H_WIDTH]
    v = u[..., 2 * H_WIDTH:]
    s = (x1 * v).astype(jnp.float32)
    kern = hyena_filter(L, f_w1, f_b1, f_w2, f_b2, f_w3, f_freq)
    n_fft = 2 * L
    y = jnp.fft.irfft(jnp.fft.rfft(s, n=n_fft, axis=1) * jnp.fft.rfft(kern, n=n_fft, axis=0)[None],
                      n=n_fft, axis=1)[:, :L]
    y = y + h_bias.astype(jnp.float32) * s
    return x0 * y.astype(z.dtype)


def merge_branches(z, a, hy, w_a, w_b, w_out):
    ga = jax.nn.sigmoid(z[..., GA0:GB0])
    gb = jax.nn.sigmoid(z[..., GB0:IN_COLS])
    return (ga * (a @ w_a) + gb * (hy @ w_b)) @ w_out


def hier_moe(h, w_group, b_group, w_router, b_router, w1_e, w3_e, w2_e):
    bsz, L, d = h.shape
    n_tok = bsz * L
    xt = h.reshape(n_tok, d)
    f32 = jnp.float32
    glog = (xt @ w_group).astype(f32) + b_group.astype(f32)
    gprob = jax.nn.softmax(glog, axis=-1)
    gsel = jnp.argmax(glog, axis=-1)
    gw = jnp.take_along_axis(gprob, gsel[:, None], axis=1)
    elog = ((xt @ w_router).astype(f32) + b_router.astype(f32)).reshape(n_tok, N_GROUPS, EXPERTS_PER_GROUP)
    elog = jnp.take_along_axis(elog, gsel[:, None, None], axis=1)[:, 0]
    eprob = jax.nn.softmax(elog, axis=-1)
    topv, topi = lax.top_k(eprob, TOP_K_IN_GROUP)
    weights = gw * topv / jnp.sum(topv, axis=-1, keepdims=True)
    experts = gsel[:, None].astype(jnp.int32) * EXPERTS_PER_GROUP + topi.astype(jnp.int32)

    n_assign = n_tok * TOP_K_IN_GROUP
    e_flat = experts.reshape(n_assign)
    w_flat = weights.reshape(n_assign)
    tok = jnp.repeat(jnp.arange(n_tok, dtype=jnp.int32), TOP_K_IN_GROUP)
    order = jnp.argsort(e_flat)
    e_s, tok_s, w_s = e_flat[order], tok[order], w_flat[order]
    counts = jax.ops.segment_sum(jnp.ones_like(e_flat), e_flat, num_segments=N_EXPERTS)
    starts = jnp.cumsum(counts) - counts
    padded = ((counts + MOE_BLOCK - 1) // MOE_BLOCK) * MOE_BLOCK
    pend = jnp.cumsum(padded)
    pstart = pend - padded
    dest = pstart[e_s] + jnp.arange(n_assign, dtype=jnp.int32) - starts[e_s]
    n_blocks = -(-n_assign // MOE_BLOCK) + N_EXPERTS
    n_slots = n_blocks * MOE_BLOCK
    slot_tok = jnp.zeros((n_slots,), jnp.int32).at[dest].set(tok_s)
    slot_w = jnp.zeros((n_slots,), f32).at[dest].set(w_s)
    block_e = jnp.clip(jnp.searchsorted(pend, jnp.arange(n_blocks, dtype=jnp.int32) * MOE_BLOCK,
                                        side='right'), 0, N_EXPERTS - 1)
    xs = xt[slot_tok].reshape(n_blocks, MOE_BLOCK, d)

    def expert_block(args):
        xb, e = args
        return (jax.nn.silu(xb @ w1_e[e]) * (xb @ w3_e[e])) @ w2_e[e]

    ys = lax.map(expert_block, (xs, block_e)).reshape(n_slots, d)
    y = jnp.zeros((n_tok, d), f32).at[slot_tok].add(ys.astype(f32) * slot_w[:, None])
    return y.astype(h.dtype).reshape(bsz, L, d)


def trunk_layer(x, xc, rows, c, c_ctx, w_mod, b_mod, g_norm1, g_norm2, w_in, b_in, w_qk_conv, b_qk_conv,
                w_h_conv, b_h_conv, hf_w1, hf_b1, hf_w2, hf_b2, hf_w3, hf_freq, h_bias, w_a, w_b, w_out,
                w_group, b_group, w_router, b_router, w1_e, w3_e, w2_e, last):
    mod = adaln(c, w_mod, b_mod)[:, None]
    modc = adaln(c_ctx, w_mod, b_mod)[None, None]
    bsz = xc.shape[0]
    f32 = jnp.float32
    hc = modulate(xc, g_norm1, modc, 0)
    cols = M_COLS if last else IN_COLS
    zc = hc @ w_in[:, :cols] + b_in[:cols]
    zero_state = (jnp.zeros((bsz, 2 * M_HEADS, M_HEAD_DIM, M_HEAD_DIM), f32),
                  jnp.zeros((bsz, 2 * M_HEADS, M_HEAD_DIM), f32),
                  jnp.zeros((bsz, 2 * M_HEADS), f32))
    ac, ctx_state = mlstm_branch(zc, w_qk_conv, b_qk_conv, 1, zero_state)
    h = modulate(x, g_norm1, mod, 0)
    z = h @ w_in + b_in
    a, _ = mlstm_branch(z, w_qk_conv, b_qk_conv, rows, ctx_state)
    hy = hyena_branch(z, w_h_conv, b_h_conv, hf_w1, hf_b1, hf_w2, hf_b2, hf_w3, hf_freq, h_bias, rows)
    x = x + mod[..., 2, :] * merge_branches(z, a, hy, w_a, w_b, w_out)
    h2 = modulate(x, g_norm2, mod, 3)
    x = x + mod[..., 5, :] * hier_moe(h2, w_group, b_group, w_router, b_router, w1_e, w3_e, w2_e)
    if not last:
        hyc = hyena_branch(zc, w_h_conv, b_h_conv, hf_w1, hf_b1, hf_w2, hf_b2, hf_w3, hf_freq, h_bias, 1)
        xc = xc + modc[..., 2, :] * merge_branches(zc, ac, hyc, w_a, w_b, w_out)
        hc2 = modulate(xc, g_norm2, modc, 3)
        xc = xc + modc[..., 5, :] * hier_moe(hc2, w_group, b_group, w_router, b_router, w1_e, w3_e, w2_e)
    return x, xc


def setup_inputs(seed: int = 0) -> dict:
    key = jax.random.key(seed)
    ks = jax.random.split(key, 40)

    def nrm(k, shape, s):
        return jax.random.normal(k, shape, jnp.float32) * s

    forget_bias = jnp.tile(jnp.linspace(3.0, 6.0, M_HEADS, dtype=jnp.float32), 2)
    return {
        'x': nrm(ks[0], (BATCH, SEQ, D_MODEL), 1.0),
        'c': nrm(ks[1], (BATCH, D_MODEL), 1.0),
        'ctx': nrm(ks[2], (BATCH, CTX_LEN, D_MODEL), 1.0),
        'c_ctx': nrm(ks[3], (D_MODEL,), 1.0),
        'w_mod': nrm(ks[4], (DEPTH, D_MODEL, 6 * D_MODEL), 0.5 * D_MODEL ** -0.5),
        'b_mod': nrm(ks[5], (DEPTH, 6 * D_MODEL), 0.02),
        'g_norm1': 1.0 + nrm(ks[6], (DEPTH, D_MODEL), 0.02),
        'g_norm2': 1.0 + nrm(ks[7], (DEPTH, D_MODEL), 0.02),
        'w_in': nrm(ks[8], (DEPTH, D_MODEL, IN_COLS), D_MODEL ** -0.5),
        'b_in': nrm(ks[9], (DEPTH, IN_COLS), 0.02).at[:, FG0:M_COLS].add(forget_bias),
        'w_qk_conv': nrm(ks[10], (DEPTH, SHORT_CONV, 2 * M_WIDTH), SHORT_CONV ** -0.5),
        'b_qk_conv': nrm(ks[11], (DEPTH, 2 * M_WIDTH), 0.02),
        'w_h_conv': nrm(ks[12], (DEPTH, SHORT_CONV, 3 * H_WIDTH), SHORT_CONV ** -0.5),
        'b_h_conv': nrm(ks[13], (DEPTH, 3 * H_WIDTH), 0.02),
        'hf_w1': nrm(ks[14], (DEPTH, H_POS_DIM, H_FILTER_HIDDEN), H_POS_DIM ** -0.5),
        'hf_b1': nrm(ks[15], (DEPTH, H_FILTER_HIDDEN), 0.1),
        'hf_w2': nrm(ks[16], (DEPTH, H_FILTER_HIDDEN, H_FILTER_HIDDEN), H_FILTER_HIDDEN ** -0.5),
        'hf_b2': nrm(ks[17], (DEPTH, H_FILTER_HIDDEN), 0.1),
        'hf_w3': nrm(ks[18], (DEPTH, H_FILTER_HIDDEN, 2 * H_WIDTH), H_FILTER_HIDDEN ** -0.5),
        'hf_freq': 1.0 + nrm(ks[19], (DEPTH, H_FILTER_HIDDEN), 0.02),
        'h_bias': nrm(ks[20], (DEPTH, H_WIDTH), 1.0),
        'w_a': nrm(ks[21], (DEPTH, M_WIDTH, D_MODEL), M_WIDTH ** -0.5),
        'w_b': nrm(ks[22], (DEPTH, H_WIDTH, D_MODEL), H_WIDTH ** -0.5),
        'w_out': nrm(ks[23], (DEPTH, D_MODEL, D_MODEL), D_MODEL ** -0.5),
        'w_group': nrm(ks[24], (DEPTH, D_MODEL, N_GROUPS), D_MODEL ** -0.5),
        'b_group': nrm(ks[25], (DEPTH, N_GROUPS), 0.01),
        'w_router': nrm(ks[26], (DEPTH, D_MODEL, N_EXPERTS), D_MODEL ** -0.5),
        'b_router': nrm(ks[27], (DEPTH, N_EXPERTS), 0.01),
        'w1_e': nrm(ks[28], (DEPTH, N_EXPERTS, D_MODEL, D_EXPERT), D_MODEL ** -0.5),
        'w3_e': nrm(ks[29], (DEPTH, N_EXPERTS, D_MODEL, D_EXPERT), D_MODEL ** -0.5),
        'w2_e': nrm(ks[30], (DEPTH, N_EXPERTS, D_EXPERT, D_MODEL), D_EXPERT ** -0.5),
        'g_final': 1.0 + nrm(ks[31], (D_MODEL,), 0.02),
    }


def reference(x, c, ctx, c_ctx, w_mod, b_mod, g_norm1, g_norm2, w_in, b_in, w_qk_conv, b_qk_conv,
              w_h_conv, b_h_conv, hf_w1, hf_b1, hf_w2, hf_b2, hf_w3, hf_freq, h_bias, w_a, w_b, w_out,
              w_group, b_group, w_router, b_router, w1_e, w3_e, w2_e, g_final):
    rows = x.shape[1] // GRID_W
    xc = ctx
    for l in range(DEPTH):
        x, xc = trunk_layer(x, xc, rows, c, c_ctx, w_mod[l], b_mod[l], g_norm1[l], g_norm2[l], w_in[l],
                            b_in[l], w_qk_conv[l], b_qk_conv[l], w_h_conv[l], b_h_conv[l], hf_w1[l],
                            hf_b1[l], hf_w2[l], hf_b2[l], hf_w3[l], hf_freq[l], h_bias[l], w_a[l], w_b[l],
                            w_out[l], w_group[l], b_group[l], w_router[l], b_router[l], w1_e[l], w3_e[l],
                            w2_e[l], l == DEPTH - 1)
    return rmsnorm(x, g_final)
```

```python
import contextlib
import math

import numpy as np
import ml_dtypes

import concourse.bass as bass
import concourse.mybir as mybir
from concourse.bass_utils import run_bass_kernel_spmd

F32 = mybir.dt.float32
BF16 = mybir.dt.bfloat16
I32 = mybir.dt.int32
U8 = mybir.dt.uint8
AF = mybir.ActivationFunctionType
ALU = mybir.AluOpType
AX = mybir.AxisListType

D = 1024
L = 8192
CTX = 256
T = L + CTX
NCH = T // 128
NQ = 2048
IN_COLS = 9232
V0, O0, IG0, FG0, HY0, GA0, GB0 = 2048, 3072, 4096, 4104, 4112, 7184, 8208
NFFT = 2 * L
EPS = 1e-6


class Sched:
    ENGS = ("sync", "scalar", "vector", "gpsimd", "tensor")

    def __init__(self, nc):
        self.nc = nc
        self.ops = []
        self.last_w = {}
        self.readers = {}
        self.extra = {e: set() for e in self.ENGS}
        self.last_on = {}
        self.dmas_since_barrier = []
        self.barrier_at = []

    def _add(self, eng, fn, reads, writes, dma):
        i = len(self.ops)
        deps = set(self.extra[eng])
        self.extra[eng] = set()
        for k in list(reads) + list(writes):
            if k in self.last_w:
                deps.add(self.last_w[k])
        for k in writes:
            rd = self.readers.get(k)
            if rd:
                deps.update(rd[0].values())
                deps.update(rd[1])
        deps.discard(i)
        for k in reads:
            rd = self.readers.setdefault(k, ({}, []))
            if dma:
                rd[1].append(i)
            else:
                rd[0][eng] = i
        for k in writes:
            self.last_w[k] = i
            self.readers[k] = ({}, [])
        self.ops.append(dict(eng=eng, fn=fn, deps=deps, dma=dma,
                             key=(writes[0] if (dma and writes) else None)))
        self.last_on[eng] = i
        if dma:
            self.dmas_since_barrier.append(i)
        return i

    def op(self, eng, fn, reads=(), writes=()):
        return self._add(eng, fn, tuple(reads), tuple(writes), False)

    def dma(self, eng, fn, reads=(), writes=()):
        assert len(writes) >= 1
        return self._add(eng, fn, tuple(reads), tuple(writes), True)

    def barrier(self):
        self.barrier_at.append(len(self.ops))
        deps = set(self.last_on.values()) | set(self.dmas_since_barrier)
        self.dmas_since_barrier = []
        for e in self.ENGS:
            self.extra[e] |= deps

    def emit(self, stack, final_wait_eng="sync"):
        nc = self.nc
        ops = self.ops
        self.barrier()
        self.op(final_wait_eng, None)
        need = set()
        for o in ops:
            for d in o["deps"]:
                od = ops[d]
                if od["dma"]:
                    continue
                if od["eng"] == "tensor" and o["eng"] == "tensor" and not o["dma"]:
                    continue
                need.add(d)
        esem = {e: stack.enter_context(nc.semaphore("se_" + e)) for e in self.ENGS}
        ksem, kcnt, sig = {}, {}, {}
        cnt = {e: 0 for e in self.ENGS}
        pool, allsems = [], []
        bset = set(self.barrier_at)
        maxk = 0
        for i, o in enumerate(ops):
            if i in bset:
                pool.extend(ksem.values())
                ksem = {}
            if o["dma"]:
                k = o["key"]
                if k not in ksem:
                    if pool:
                        ksem[k] = pool.pop()
                    else:
                        sm_ = stack.enter_context(nc.semaphore("sd_%d" % len(allsems)))
                        allsems.append(sm_)
                        kcnt[id(sm_)] = 0
                        ksem[k] = sm_
                    maxk = max(maxk, len(ksem))
                sm_ = ksem[k]
                kcnt[id(sm_)] += 16
                sig[i] = (sm_, kcnt[id(sm_)], 16)
            elif i in need:
                cnt[o["eng"]] += 1
                sig[i] = (esem[o["eng"]], cnt[o["eng"]], 1)
        self.n_sems = len(allsems) + 5
        per = {e: [] for e in self.ENGS}
        for i, o in enumerate(ops):
            per[o["eng"]].append(i)
        block = stack.enter_context(nc.Block())

        def run(eng, e):
            waited = {}
            for i in per[e]:
                o = ops[i]
                ws = {}
                for d in o["deps"]:
                    if d not in sig:
                        continue
                    s, v, _ = sig[d]
                    if v > ws.get(id(s), (None, 0))[1]:
                        ws[id(s)] = (s, v)
                for sid, (s, v) in ws.items():
                    if waited.get(sid, 0) < v:
                        eng.wait_ge(s, v)
                        waited[sid] = v
                if o["fn"] is None:
                    continue
                ins = o["fn"](eng)
                if i in sig:
                    s, v, inc = sig[i]
                    ins.then_inc(s, inc)

        @block.sync
        def _(eng):
            run(eng, "sync")

        @block.scalar
        def _(eng):
            run(eng, "scalar")

        @block.vector
        def _(eng):
            run(eng, "vector")

        @block.gpsimd
        def _(eng):
            run(eng, "gpsimd")

        @block.tensor
        def _(eng):
            run(eng, "tensor")


class Arena:
    def __init__(self, nc, stack, nbytes):
        self.t = stack.enter_context(nc.sbuf_tensor("arena", [128, nbytes], U8))
        self.nbytes = nbytes
        self.off = 0
        self.peak = 0

    def alloc(self, n, dt, parts=128):
        sz = 4 if dt in (F32, I32) else 2
        nb = n * sz
        assert self.off + nb <= self.nbytes, ("SBUF arena overflow", self.off, nb)
        v = self.t[0:parts, self.off:self.off + nb].bitcast(dt)
        self.off += (nb + 63) // 64 * 64
        self.peak = max(self.peak, self.off)
        return v

    def mark(self):
        return self.off

    def release(self, m):
        self.off = m


def v3(ap, a, b):
    return ap.rearrange("p (a b) -> p a b", a=a, b=b)


def v4(ap, a, b, c):
    return ap.rearrange("p (a b c) -> p a b c", a=a, b=b, c=c)


def host_consts():
    c = {}
    c["ident"] = np.eye(128, dtype=np.float32)
    s_le_t = (np.arange(128)[:, None] <= np.arange(128)[None, :]).astype(np.float32)
    c["masks"] = np.concatenate([s_le_t, s_le_t.T], axis=1)
    ang = 2 * np.pi * np.outer(np.arange(128), np.arange(128)) / 128.0
    c["dft"] = np.concatenate([np.cos(ang), np.sin(ang), -np.sin(ang)], axis=1).astype(np.float32)
    angn = 2 * np.pi * np.outer(np.arange(128), np.arange(128)) / float(NFFT)
    c["tw"] = np.concatenate([np.cos(angn), np.sin(angn)], axis=1).astype(np.float32)
    n = np.arange(NFFT)
    pos = np.where(n < L, n, NFFT - n).astype(np.float64)
    t = pos / (L - 1)
    bands = np.linspace(1e-4, 16 - 1, 16).astype(np.float32).astype(np.float64)
    ang2 = (2 * math.pi / L) * pos[:, None] * bands[None]
    feats = np.concatenate([t[:, None], np.cos(ang2), -np.sin(ang2)], axis=-1)
    c["featsT"] = np.ascontiguousarray(feats.T).astype(np.float32)
    c["negt"] = np.ascontiguousarray((-t).reshape(128, 128)).astype(np.float32)
    max_decay = math.log(1e-2) / 0.3
    min_decay = math.log(1e-2) / 1.5
    c["deltas"] = np.abs(np.linspace(min_decay, max_decay, 1024, dtype=np.float32)).astype(np.float32)
    sel = np.zeros((36, 8), np.float32)
    for g in range(4):
        sel[g, g] = 1.0
        sel[32 + g, 4 + g] = 1.0
    c["sel"] = sel
    return c


def build_nc(upto="all", debug=(), skip=()):
    nc = bass.Bass("TRN2", target_bir_lowering=False)
    dbg = set(debug)

    in_names = []
    nc._in_names = in_names
    full = upto == "all"

    def din(name, shape, dt=F32, big=False):
        if big and not full:
            return None
        in_names.append(name)
        return nc.dram_tensor(name, list(shape), dt, kind="ExternalInput").ap()

    def dscr(name, shape, dt):
        kind = "ExternalOutput" if name in dbg else "Internal"
        return nc.dram_tensor(name, list(shape), dt, kind=kind).ap()

    xb = din("xb", [L, D])
    ctxb = din("ctxb", [CTX, D])
    xq = din("xq", [NQ, D])
    idxq = din("idxq", [128, 16], I32)
    cT_d = din("cT", [128, 16])
    w_mod = din("w_mod", [D, 6 * D])
    b_mod = din("b_mod", [1, 6 * D])
    g1_d = din("g1", [D])
    g2_d = din("g2", [D])
    w_in = din("w_in", [D, IN_COLS])
    b_in = din("b_in", [IN_COLS])
    qkpar_d = din("qkpar", [128, 16 * 5])
    hypar_d = din("hypar", [128, 24 * 5])
    gpar_d = din("gpar", [36, 2])
    hf_w1 = din("hf_w1", [33, 64])
    hfpar_d = din("hfpar", [128, 4])
    hf_w2 = din("hf_w2", [64, 64])
    hf_w3 = din("hf_w3", [64, 2048])
    h_bias = din("h_bias", [D])
    w_a = din("w_a", big=True, shape=[D, D])
    w_b = din("w_b", big=True, shape=[D, D])
    w_out = din("w_out", big=True, shape=[D, D])
    w_rt = din("w_rt", [D, 72])
    b_rt = din("b_rt", [72])
    w1_e = din("w1_e", big=True, shape=[64, D, 512])
    w3_e = din("w3_e", big=True, shape=[64, D, 512])
    w2_e = din("w2_e", big=True, shape=[64, 512, D])
    g_fin = din("g_fin", [D])
    c_ident = din("ident", [128, 128])
    c_masks = din("masks", [128, 256])
    c_dft = din("dft", [128, 384])
    c_tw = din("tw", [128, 256])
    c_featsT = din("featsT", [33, NFFT])
    c_negt = din("negt", [128, 128])
    c_deltas = din("deltas", [D])
    c_sel = din("sel", [36, 8])
    out = nc.dram_tensor("out", [NQ, D], F32, kind="ExternalOutput").ap()

    hT_d = dscr("hT_d", [128, 8, T], BF16)
    QK_d = dscr("QK_d", [16, 128, T], BF16)
    V_d = dscr("V_d", [T, D], BF16)
    HC_d = dscr("HC_d", [L, 3 * D], BF16)
    HF_d = HC_d[:, 0:D]
    HB_d = HC_d[:, D:2 * D]
    S_d = dscr("S_d", [L, D], BF16)
    X0_d = dscr("X0_d", [L, D], BF16)
    HY_d = HC_d[:, 2 * D:3 * D]
    G_d = dscr("G_d", [2, 2, 128, 128, 128], BF16)
    KH_d = dscr("KH_d", [2, 128, 128, 128], BF16)
    H_d = dscr("H_d", [2, 128, 128, 128], BF16)
    X1_d = dscr("X1_d", [NQ, D], F32)
    IG_d = dscr("IG_d", [36, T], F32)
    OG_d = dscr("OG_d", [NQ, 3 * D], BF16)
    H2T_d = dscr("H2T_d", [128, 8, NQ], BF16)
    WGT_d = dscr("WGT_d", [128, 16 * 64], F32)
    MOE_d = dscr("MOE_d", [NQ, D], F32)
    FG_d = dscr("FG_d", [36, T], F32)
    KERN_d = dscr("KERN_d", [128, 128, 128], BF16)
    GATE_d = dscr("GATE_d", [128, 4 * NCH * 8], F32)

    w_in_v = w_in.rearrange("(k p) n -> p k n", p=128)

    with contextlib.ExitStack() as st:
        S = Sched(nc)
        A = Arena(nc, st, 203 * 1024)
        ps = [st.enter_context(nc.psum_tensor("ps%d" % i, [128, 512], F32)) for i in range(7)]
        pb = st.enter_context(nc.psum_tensor("pb", [128, 1024], BF16))
        PS = ["ps%d" % i for i in range(7)]

        def MM(o, lhsT, rhs, start, stop, r, w):
            S.op("tensor", lambda e: e.matmul(o, lhsT=lhsT, rhs=rhs, start=start, stop=stop), r, w)

        def TR(o, i, ident, r, w):
            S.op("tensor", lambda e: e.transpose(o, i, ident), r, w)

        def ACT(o, i, func, r, w, bias=None, scale=None, accum=None):
            kw = {}
            if bias is not None:
                kw["bias"] = bias
            if scale is not None:
                kw["scale"] = scale
            if accum is not None:
                kw["accum_out"] = accum
            S.op("scalar", lambda e: e.activation(out=o, in_=i, func=func, **kw), r, w)

        def TT(eng, o, a, b, op, r, w):
            S.op(eng, lambda e: e.tensor_tensor(out=o, in0=a, in1=b, op=op), r, w)

        def TS(eng, o, a, s1, op0, r, w, s2=None, op1=None):
            if op1 is None:
                S.op(eng, lambda e: e.tensor_scalar(out=o, in0=a, scalar1=s1, scalar2=None, op0=op0), r, w)
            else:
                S.op(eng, lambda e: e.tensor_scalar(out=o, in0=a, scalar1=s1, scalar2=s2, op0=op0, op1=op1), r, w)

        def STT(eng, o, a, s, b, op0, op1, r, w):
            S.op(eng, lambda e: e.scalar_tensor_tensor(out=o, in0=a, scalar=s, in1=b, op0=op0, op1=op1), r, w)

        def CP(eng, o, i, r, w):
            if eng == "scalar":
                S.op(eng, lambda e: e.copy(out=o, in_=i), r, w)
            else:
                S.op(eng, lambda e: e.tensor_copy(out=o, in_=i), r, w)

        def MS(eng, o, val, w):
            S.op(eng, lambda e: e.memset(o, val), (), w)

        def DMA(eng, o, i, r, w):
            S.dma(eng, lambda e: e.dma_start(out=o, in_=i), r, w)

        def RED(eng, o, i, op, r, w, axis=AX.X):
            S.op(eng, lambda e: e.tensor_reduce(out=o, in_=i, axis=axis, op=op), r, w)

        def DBG(name, ap, shape, dt, r):
            if name in dbg:
                t_ = nc.dram_tensor(name, list(shape), dt, kind="ExternalOutput").ap()
                DMA("sync", t_, ap, r, [name])

        def RCP(o, i, r, w):
            S.op("vector", lambda e: e.reciprocal(out=o, in_=i), r, w)

        ident32 = A.alloc(128, F32)
        identb = A.alloc(128, BF16)
        maskb = A.alloc(256, BF16)
        dftb = A.alloc(384, BF16)
        tw = A.alloc(256, F32)
        ones32 = A.alloc(128, F32)
        onesb = A.alloc(128, BF16)
        epst = A.alloc(1, F32)
        stg = A.alloc(384, F32)
        DMA("sync", ident32, c_ident[:, :], [], ["ident32"])
        CP("vector", identb, ident32, ["ident32"], ["identb"])
        DMA("sync", stg[:, 0:256], c_masks[:, :], [], ["stg"])
        CP("vector", maskb, stg[:, 0:256], ["stg"], ["maskb"])
        DMA("sync", stg, c_dft[:, :], ["maskb"], ["stg"])
        CP("vector", dftb, stg, ["stg"], ["dftb"])
        DMA("sync", tw, c_tw[:, :], [], ["tw"])
        MS("vector", ones32, 1.0, ["ones32"])
        MS("vector", onesb, 1.0, ["onesb"])
        MS("vector", epst, EPS, ["epst"])
        Cm, Sm_, nSm = dftb[:, 0:128], dftb[:, 128:256], dftb[:, 256:384]
        GT2 = A.alloc(D, F32)
        mark_low = A.mark()
        GT1 = A.alloc(D, F32)
        G2 = A.alloc(D, F32)
        SH2 = A.alloc(D, F32)
        G1 = A.alloc(D, F32)
        SH1 = A.alloc(D, F32)

        def stageA():
            m = A.mark()
            G1c = A.alloc(D, F32)
            SH1c = A.alloc(D, F32)
            cT = A.alloc(16, F32)
            bmod = A.alloc(6 * D, F32, parts=1)
            modrow = [A.alloc(6 * D, F32, parts=1) for _ in range(2)]
            wm = [A.alloc(8 * 512, F32) for _ in range(2)]
            gbc = A.alloc(D, F32)
            DMA("sync", cT, cT_d[:, :], [], ["cT"])
            ACT(cT, cT, AF.Silu, ["cT"], ["cT"])
            DMA("sync", bmod, b_mod[:, :], [], ["bmod"])
            cT3 = v3(cT, 8, 2)
            w_mod_v = w_mod.rearrange("(k p) n -> p k n", p=128)
            for n in range(12):
                buf = wm[n % 2]
                bk = "wm%d" % (n % 2)
                DMA("sync" if n % 2 == 0 else "scalar", v3(buf, 8, 512), w_mod_v[:, :, n * 512:(n + 1) * 512], [], [bk])
                for j in range(2):
                    for k in range(8):
                        MM(ps[j][0:1, :], cT3[:, k, j:j + 1], buf[:, k * 512:(k + 1) * 512], k == 0, k == 7,
                           [bk, "cT"], [PS[j]])
                    TT("vector", modrow[j][0:1, n * 512:(n + 1) * 512], ps[j][0:1, :], bmod[0:1, n * 512:(n + 1) * 512],
                       ALU.add, [PS[j], "bmod"], ["modrow%d" % j])

            def bcast(dst, dkey, j, idx):
                for h in range(2):
                    MM(ps[2 + h][:, :], ones32[0:1, :], modrow[j][0:1, idx * D + h * 512: idx * D + (h + 1) * 512],
                       True, True, ["modrow%d" % j, "ones32"], [PS[2 + h]])
                    CP("vector", dst[:, h * 512:(h + 1) * 512], ps[2 + h][:, :], [PS[2 + h]], [dkey])

            bcast(SH1, "SH1", 0, 0)
            bcast(G1, "G1", 0, 1)
            bcast(GT1, "GT1", 0, 2)
            bcast(SH2, "SH2", 0, 3)
            bcast(G2, "G2", 0, 4)
            bcast(GT2, "GT2", 0, 5)
            bcast(SH1c, "SH1c", 1, 0)
            bcast(G1c, "G1c", 1, 1)
            DMA("sync", gbc, g1_d.partition_broadcast(128), [], ["gbc"])
            STT("vector", G1, G1, 1.0, gbc, ALU.add, ALU.mult, ["G1", "gbc"], ["G1"])
            STT("vector", G1c, G1c, 1.0, gbc, ALU.add, ALU.mult, ["G1c", "gbc"], ["G1c"])
            DMA("sync", gbc, g2_d.partition_broadcast(128), ["G1", "G1c"], ["gbc"])
            STT("vector", G2, G2, 1.0, gbc, ALU.add, ALU.mult, ["G2", "gbc"], ["G2"])
            for nm_, t_ in (("dG1", G1), ("dSH1", SH1), ("dGT1", GT1), ("dG2", G2), ("dSH2", SH2), ("dGT2", GT2), ("dG1c", G1c), ("dSH1c", SH1c)):
                DBG(nm_, t_, [128, D], F32, [nm_[1:]])
            A.release(m)
            A.off = m
            A.alloc(D, F32)
            A.alloc(D, F32)
            return G1c, SH1c

        nrm = {}

        def norm_setup(nb=3, with_x=True):
            nrm["nb"] = nb
            nrm["xt"] = [A.alloc(D, F32) for _ in range(nb)] if with_x else None
            nrm["junk"] = [A.alloc(D, BF16) for _ in range(nb)]
            nrm["t1"] = [A.alloc(D, F32) for _ in range(nb)]
            nrm["st"] = [A.alloc(8, F32) for _ in range(nb)]
            nrm["n"] = 0

        def norm_tile(src, Gt, Gk, SHt, SHk, hb, hbk, x_keep=None):
            i = nrm["n"] % nrm["nb"]
            nrm["n"] += 1
            xt = nrm["xt"][i] if x_keep is None else x_keep[0]
            xk = ("nxt%d" % i) if x_keep is None else x_keep[1]
            stt = nrm["st"][i]
            sk_, jk_, tk_ = "nst%d" % i, "njunk%d" % i, "nt1%d" % i
            if src is not None:
                DMA("sync", xt, src, [], [xk])
            MS("vector", stt[:, 0:1], 0.0, [sk_])
            ACT(nrm["junk"][i], xt, AF.Square, [xk, sk_], [jk_, sk_], accum=stt[:, 0:1])
            ACT(stt[:, 1:2], stt[:, 0:1], AF.Sqrt, [sk_, "epst"], [sk_], bias=epst[:, 0:1], scale=1.0 / D)
            RCP(stt[:, 2:3], stt[:, 1:2], [sk_], [sk_])
            STT("vector", nrm["t1"][i], xt, stt[:, 2:3], Gt, ALU.mult, ALU.mult, [xk, sk_, Gk], [tk_])
            TT("gpsimd", hb, nrm["t1"][i], SHt, ALU.add, [tk_, SHk], [hbk])

        def stageB(G1c, SH1c):
            m = A.mark()
            norm_setup()
            hb = [A.alloc(D, BF16) for _ in range(3)]
            hTt = [A.alloc(D, BF16) for _ in range(3)]
            for tt in range(NCH):
                i = tt % 3
                src = ctxb[tt * 128:(tt + 1) * 128, :] if tt < 2 else xb[(tt - 2) * 128:(tt - 1) * 128, :]
                if tt < 2:
                    norm_tile(src, G1c, "G1c", SH1c, "SH1c", hb[i], "hb%d" % i)
                else:
                    norm_tile(src, G1, "G1", SH1, "SH1", hb[i], "hb%d" % i)
                for k in range(8):
                    TR(pb[:, k * 128:(k + 1) * 128], hb[i][:, k * 128:(k + 1) * 128], identb, ["hb%d" % i, "identb"], ["pb"])
                CP("scalar", hTt[i], pb[:, :], ["pb"], ["hTt%d" % i])
                DMA("scalar", hT_d[:, :, tt * 128:(tt + 1) * 128], v3(hTt[i], 8, 128), ["hTt%d" % i], ["hT_d%d" % (tt % 4)])
            A.release(m)
            S.barrier()

        def load_w_bf16(dst3, dkey, cols, n, stage, skey, eng="sync"):
            DMA(eng, v3(stage[:, 0:8 * n], 8, n), w_in_v[:, :, cols:cols + n], [], [skey])
            CP("gpsimd", dst3, v3(stage[:, 0:8 * n], 8, n), [skey], [dkey])

        def stageC1():
            m = A.mark()
            gst = [A.alloc(512, F32) for _ in range(2)]
            stage = [A.alloc(8 * 512, F32) for _ in range(2)]
            wqk = A.alloc(8 * 2048, BF16)
            wv = A.alloc(8 * 1024, BF16)
            wg = A.alloc(8 * 72, BF16)
            wg32 = A.alloc(8 * 72, F32)
            qkpar = A.alloc(80, F32)
            gpar = A.alloc(2, F32)
            bv = A.alloc(1024, F32)
            DMA("sync", qkpar, qkpar_d[:, :], [], ["qkpar"])
            DMA("sync", gpar[0:36, :], gpar_d[:, :], [], ["gpar"])
            DMA("sync", bv, b_in[V0:V0 + 1024].partition_broadcast(128), [], ["bv"])
            wqk3 = v3(wqk, 8, 2048)
            for g in range(4):
                load_w_bf16(wqk3[:, :, g * 512:(g + 1) * 512], "wqk", g * 512, 512, stage[g % 2], "stage%d" % (g % 2),
                            "sync" if g % 2 == 0 else "scalar")
            wv3 = v3(wv, 8, 1024)
            for g in range(2):
                load_w_bf16(wv3[:, :, g * 512:(g + 1) * 512], "wv", V0 + g * 512, 512, stage[g % 2], "stage%d" % (g % 2),
                            "sync" if g % 2 == 0 else "scalar")
            MS("vector", wg32, 0.0, ["wg32"])
            wg323 = v3(wg32, 8, 72)
            with nc.allow_non_contiguous_dma(reason="tiny gate weight columns"):
                DMA("sync", wg323[:, :, 0:4], w_in_v[:, :, IG0:IG0 + 4], ["wg32"], ["wg32"])
                DMA("sync", wg323[:, :, 32:36], w_in_v[:, :, IG0 + 4:IG0 + 8], ["wg32"], ["wg32"])
                DMA("sync", wg323[:, :, 36:40], w_in_v[:, :, FG0:FG0 + 4], ["wg32"], ["wg32"])
                DMA("sync", wg323[:, :, 68:72], w_in_v[:, :, FG0 + 4:FG0 + 8], ["wg32"], ["wg32"])
            CP("vector", wg, wg32, ["wg32"], ["wg"])
            wg3 = v3(wg, 8, 72)
            hTb = [A.alloc(8 * 512, BF16) for _ in range(2)]
            zf = [A.alloc(512, F32) for _ in range(4)]
            yf = [A.alloc(512, F32) for _ in range(4)]
            qo = [A.alloc(512, BF16) for _ in range(3)]
            vo = [A.alloc(512, BF16) for _ in range(2)]
            qkbank = (0, 1, 5, 6)
            par3 = v3(qkpar, 16, 5)
            nq = 0
            nv = 0
            def c1_load(tb):
                t0_ = 0 if tb == 0 else CTX + (tb - 1) * 512
                n_ = CTX if tb == 0 else 512
                DMA("sync", v3(hTb[tb % 2], 8, 512)[:, :, 0:n_], hT_d[:, :, t0_:t0_ + n_], ["hT_d0", "hT_d1", "hT_d2", "hT_d3"],
                    ["hTb%d" % (tb % 2)])

            c1_load(0)
            for tb in range(17):
                t0 = 0 if tb == 0 else CTX + (tb - 1) * 512
                n = CTX if tb == 0 else 512
                hb_ = hTb[tb % 2]
                hk = "hTb%d" % (tb % 2)
                h3 = v3(hb_, 8, 512)
                if tb + 1 < 17:
                    c1_load(tb + 1)
                rows, rl = (1, CTX) if tb == 0 else (8, 64)
                for ct in range(16):
                    bi = ct % 4
                    pi = qkbank[bi]
                    for k in range(8):
                        MM(ps[pi][:, 0:n], wqk3[:, k, ct * 128:(ct + 1) * 128], h3[:, k, 0:n], k == 0, k == 7,
                           ["wqk", hk], [PS[pi]])
                    z = zf[bi]
                    y = yf[bi]
                    zk, yk = "zf%d" % bi, "yf%d" % bi
                    ACT(z[:, 0:n], ps[pi][:, 0:n], AF.Identity, [PS[pi], "qkpar"], [zk], bias=par3[:, ct, 4:5])
                    ACT(y[:, 0:n], z[:, 0:n], AF.Identity, [zk, "qkpar"], [yk], bias=par3[:, ct, 3:4], scale=par3[:, ct, 1:2])
                    z3 = v3(z[:, 0:n], rows, rl)
                    y3 = v3(y[:, 0:n], rows, rl)
                    STT("vector", y3[:, :, 1:rl], z3[:, :, 0:rl - 1], par3[:, ct, 0:1], y3[:, :, 1:rl], ALU.mult, ALU.add,
                        [zk, yk, "qkpar"], [yk])
                    STT("vector", y3[:, :, 0:rl - 1], z3[:, :, 1:rl], par3[:, ct, 2:3], y3[:, :, 0:rl - 1], ALU.mult, ALU.add,
                        [zk, yk, "qkpar"], [yk])
                    q = qo[nq % 3]
                    qk_ = "qo%d" % (nq % 3)
                    nq += 1
                    ACT(q[:, 0:n], y[:, 0:n], AF.Silu, [yk], [qk_])
                    DMA("scalar", QK_d[ct, :, t0:t0 + n], q[:, 0:n], [qk_], ["QK_d%d" % (ct % 4)])
                for gi, GTd in enumerate((IG_d, FG_d)):
                    for k in range(8):
                        MM(ps[2][0:36, 0:n], wg3[:, k, gi * 36:(gi + 1) * 36], h3[:, k, 0:n], k == 0, k == 7,
                           ["wg", hk], [PS[2]])
                    ACT(gst[gi][0:36, 0:n], ps[2][0:36, 0:n], AF.Identity, [PS[2], "gpar"], ["gst%d" % gi], bias=gpar[0:36, gi:gi + 1])
                    DMA("scalar", GTd[:, t0:t0 + n], gst[gi][0:36, 0:n], ["gst%d" % gi], ["G%d_d" % gi])
                for tt in range(n // 128):
                    for g in range(2):
                        pi = 3 + g
                        for k in range(8):
                            MM(ps[pi][:, :], h3[:, k, tt * 128:(tt + 1) * 128], wv3[:, k, g * 512:(g + 1) * 512], k == 0, k == 7,
                               ["wv", hk], [PS[pi]])
                        o_ = vo[nv % 2]
                        ok = "vo%d" % (nv % 2)
                        nv += 1
                        TT("vector", o_, ps[pi][:, :], bv[:, g * 512:(g + 1) * 512], ALU.add, [PS[pi], "bv"], [ok])
                        DMA("scalar", V_d[t0 + tt * 128:t0 + (tt + 1) * 128, g * 512:(g + 1) * 512], o_, [ok], ["V_d%d" % (nv % 4)])
            A.release(m)
            S.barrier()

        def stageC2():
            m = A.mark()
            stage = [A.alloc(8 * 512, F32) for _ in range(2)]
            wh = A.alloc(8 * 3072, BF16)
            wh3 = v3(wh, 8, 3072)
            hypar = A.alloc(120, F32)
            DMA("sync", hypar, hypar_d[:, :], [], ["hypar"])
            par3 = v3(hypar, 24, 5)
            for g in range(6):
                load_w_bf16(wh3[:, :, g * 512:(g + 1) * 512], "wh", HY0 + g * 512, 512, stage[g % 2], "stage%d" % (g % 2),
                            "sync" if g % 2 == 0 else "scalar")
            hTb = [A.alloc(8 * 512, BF16) for _ in range(2)]
            zf = [A.alloc(512, F32) for _ in range(3)]
            yf = [A.alloc(512, F32) for _ in range(3)]
            sb_ = [A.alloc(512, BF16) for _ in range(2)]
            x0b = [A.alloc(512, BF16) for _ in range(2)]
            tok = [A.alloc(1024, BF16) for _ in range(2)]
            cnt = 0
            def c2_load(tb):
                DMA("sync", v3(hTb[tb % 2], 8, 512), hT_d[:, :, CTX + tb * 512:CTX + (tb + 1) * 512], [], ["hTb%d" % (tb % 2)])

            c2_load(0)
            for tb in range(16):
                t0 = CTX + tb * 512
                hb_ = hTb[tb % 2]
                hk = "hTb%d" % (tb % 2)
                h3 = v3(hb_, 8, 512)
                if tb + 1 < 16:
                    c2_load(tb + 1)
                for j in range(8):
                    i2 = cnt % 2
                    cnt += 1
                    for part in range(3):
                        ct = part * 8 + j
                        pi = part
                        for k in range(8):
                            MM(ps[pi][:, :], wh3[:, k, ct * 128:(ct + 1) * 128], h3[:, k, :], k == 0, k == 7, ["wh", hk], [PS[pi]])
                        z, y = zf[part], yf[part]
                        zk, yk = "hzf%d" % part, "hyf%d" % part
                        ACT(z, ps[pi][:, :], AF.Identity, [PS[pi], "hypar"], [zk], bias=par3[:, ct, 4:5])
                        ACT(y, z, AF.Identity, [zk, "hypar"], [yk], bias=par3[:, ct, 3:4], scale=par3[:, ct, 1:2])
                        z3, y3 = v3(z, 8, 64), v3(y, 8, 64)
                        STT("vector", y3[:, :, 1:64], z3[:, :, 0:63], par3[:, ct, 0:1], y3[:, :, 1:64], ALU.mult, ALU.add,
                            [zk, yk, "hypar"], [yk])
                        STT("vector", y3[:, :, 0:63], z3[:, :, 1:64], par3[:, ct, 2:3], y3[:, :, 0:63], ALU.mult, ALU.add,
                            [zk, yk, "hypar"], [yk])
                    CP("scalar", x0b[i2], yf[0], ["hyf0"], ["x0b%d" % i2])
                    TT("gpsimd", sb_[i2], yf[1], yf[2], ALU.mult, ["hyf1", "hyf2"], ["sb%d" % i2])
                    for q in range(4):
                        TR(pb[:, q * 128:(q + 1) * 128], sb_[i2][:, q * 128:(q + 1) * 128], identb, ["sb%d" % i2, "identb"], ["pb"])
                        TR(pb[:, 512 + q * 128:512 + (q + 1) * 128], x0b[i2][:, q * 128:(q + 1) * 128], identb,
                           ["x0b%d" % i2, "identb"], ["pb"])
                    CP("scalar", tok[i2], pb[:, :], ["pb"], ["tok%d" % i2])
                    tk3 = v3(tok[i2], 8, 128)
                    DMA("scalar", S_d[tb * 512:tb * 512 + 512, j * 128:(j + 1) * 128].rearrange("(q p) c -> p q c", p=128), tk3[:, 0:4, :],
                        ["tok%d" % i2], ["S_d%d" % (cnt % 4)])
                    DMA("scalar", X0_d[tb * 512:tb * 512 + 512, j * 128:(j + 1) * 128].rearrange("(q p) c -> p q c", p=128), tk3[:, 4:8, :],
                        ["tok%d" % i2], ["X0_d%d" % (cnt % 4)])
            A.release(m)
            S.barrier()

        def gates_pre(Wt, EMTt, DECb):
            m = A.mark()
            IGT = A.alloc(T, F32)
            FGT = A.alloc(T, F32)
            NB = A.alloc(T, F32)
            G = NB
            base = A.alloc(NCH, F32)
            DMA("sync", IGT[0:36, :], IG_d[:, :], [], ["IGT"])
            DMA("scalar", FGT[0:36, :], FG_d[:, :], [], ["FGT"])
            totn = A.alloc(NCH, F32)
            mc = A.alloc(2, F32)
            dec = A.alloc(NCH, F32)
            R = A.alloc(NCH * 8, F32)
            selm = A.alloc(8, F32)
            selx = A.alloc(NCH * 8, F32)
            DMA("sync", selm[0:36, :], c_sel[:, :], [], ["selm"])
            ACT(FGT[0:36, :], FGT[0:36, :], AF.Exp, ["FGT"], ["FGT"], scale=-1.0)
            ACT(FGT[0:36, :], FGT[0:36, :], AF.Ln, ["FGT"], ["FGT"], bias=1.0)
            S.op("vector", lambda e: e.tensor_tensor_scan(out=G[0:36, :], data0=ones32[0:36, 0:1].to_broadcast([36, T]), data1=FGT[0:36, :],
                                                          initial=0.0, op0=ALU.mult, op1=ALU.add), ["ones32", "FGT"], ["NB"])
            N3 = v3(NB[0:36, :], NCH, 128)
            CP("vector", base[0:36, 0:NCH - 1], N3[:, 0:NCH - 1, 127], ["NB"], ["base"])
            TT("vector", N3[:, 1:NCH, :], N3[:, 1:NCH, :], base[0:36, 0:NCH - 1].unsqueeze(2).to_broadcast([36, NCH - 1, 128]), ALU.subtract,
               ["NB", "base"], ["NB"])
            CP("vector", totn[0:36, :], N3[:, :, 127], ["NB"], ["totn"])
            Nb = v3(NB[32:36, :], NCH, 128)
            TT("vector", Nb, totn[32:36, :].unsqueeze(2).to_broadcast([4, NCH, 128]), Nb, ALU.subtract, ["NB", "totn"], ["NB"])
            TT("vector", NB[32:36, :], NB[32:36, :], FGT[32:36, :], ALU.add, ["NB", "FGT"], ["NB"])
            TT("vector", IGT[0:36, :], IGT[0:36, :], NB[0:36, :], ALU.add, ["IGT", "NB"], ["IGT"])
            RED("vector", mc[0:36, 0:1], IGT[0:36, :], ALU.max, ["IGT"], ["mc"])
            TS("vector", mc[0:36, 1:2], mc[0:36, 0:1], -1.0, ALU.mult, ["mc"], ["mc"])
            ACT(IGT[0:36, :], IGT[0:36, :], AF.Exp, ["IGT", "mc"], ["IGT"], bias=mc[0:36, 1:2])
            TS("vector", mc[0:36, 0:1], mc[0:36, 1:2], math.log(16.0), ALU.add, ["mc"], ["mc"])
            ACT(NB[0:36, :], NB[0:36, :], AF.Exp, ["NB", "mc"], ["NB"], bias=mc[0:36, 0:1])
            ACT(dec[0:36, :], totn[0:36, :], AF.Exp, ["totn"], ["dec"], scale=-1.0)
            for src, sk, dst, dk in ((IGT, "IGT", Wt, "Wt"), (NB, "NB", EMTt, "EMTt")):
                for half in range(2):
                    for cc in range(33):
                        c = half * 33 + cc
                        MM(ps[half][:, cc * 8:(cc + 1) * 8], src[0:36, c * 128:(c + 1) * 128], selm[0:36, :], True, True,
                           [sk, "selm"], [PS[half]])
                    CP("vector", dst[:, half * 264:(half + 1) * 264], ps[half][:, 0:264], [PS[half]], [dk])
            MS("vector", R[0:36, :], 0.0, ["R"])
            CP("vector", v3(selx[0:36, :], NCH, 8), selm[0:36, :].unsqueeze(1).to_broadcast([36, NCH, 8]), ["selm"], ["selx"])
            TT("vector", v3(R[0:36, :], NCH, 8), v3(selx[0:36, :], NCH, 8), dec[0:36, :].unsqueeze(2).to_broadcast([36, NCH, 8]),
               ALU.mult, ["selx", "dec"], ["R"])
            for half in range(2):
                MM(ps[2 + half][:, 0:264], ones32[0:36, :], R[0:36, half * 264:(half + 1) * 264], True, True, ["R", "ones32"],
                   [PS[2 + half]])
                CP("vector", DECb[:, half * 264:(half + 1) * 264], ps[2 + half][:, 0:264], [PS[2 + half]], ["DECb"])
            if "GATE_d" in dbg:
                DMA("sync", GATE_d[:, 0:528], Wt, ["Wt"], ["GATE_d"])
                DMA("sync", GATE_d[:, 528:1056], EMTt, ["EMTt"], ["GATE_d"])
                DMA("sync", GATE_d[:, 1056:1584], DECb, ["DECb"], ["GATE_d"])
            A.release(m)
            S.barrier()

        def stageD():
            m0 = A.mark()
            Wt = A.alloc(NCH * 8, F32)
            EMTt = A.alloc(NCH * 8, F32)
            DECb = A.alloc(NCH * 8, F32)
            gates_pre(Wt, EMTt, DECb)
            QT = A.alloc(2 * T, BF16)
            KT = A.alloc(2 * T, BF16)
            Vx = A.alloc(NCH * 258, BF16)
            Ktok = A.alloc(NCH * 256, BF16)
            Est = [A.alloc(514, F32) for _ in range(2)]
            Cb = [[A.alloc(514, BF16) for _ in range(2)] for _ in range(2)]
            Vp = [A.alloc(257, BF16) for _ in range(4)]
            Smk = [A.alloc(128, BF16) for _ in range(4)]
            ho = [A.alloc(256, BF16) for _ in range(4)]
            sm = [A.alloc(4, F32) for _ in range(4)]
            QT3, KT3 = v3(QT, 2, T), v3(KT, 2, T)
            Vx3 = v3(Vx, NCH, 258)
            Kt3 = v3(Ktok, NCH, 256)
            order = [list(range(NCH)), [1, 0] + list(range(NCH - 1, 1, -1))]
            step = 0
            for h in range(4):
                DMA("sync", QT3, QK_d[2 * h:2 * h + 2, :, :].rearrange("c p t -> p c t"), [], ["QT"])
                DMA("scalar", KT3, QK_d[8 + 2 * h:8 + 2 * h + 2, :, :].rearrange("c p t -> p c t"), [], ["KT"])
                DMA("sync", Vx3[:, :, 0:256], V_d[:, h * 256:(h + 1) * 256].rearrange("(c p) e -> p c e", p=128), [], ["Vx"])
                MS("vector", Vx3[:, :, 256:257], 1.0, ["Vx1"])
                for c0 in range(0, NCH, 4):
                    nn = min(4, NCH - c0)
                    for cc in range(nn):
                        for dh in range(2):
                            TR(pb[:, (cc * 2 + dh) * 128:(cc * 2 + dh + 1) * 128], KT3[:, dh, (c0 + cc) * 128:(c0 + cc + 1) * 128], identb,
                               ["KT", "identb"], ["pb"])
                    CP("scalar", Ktok[:, c0 * 256:(c0 + nn) * 256], pb[:, 0:nn * 256], ["pb"], ["Ktok"])
                for d in range(2):
                    MS("vector", Est[d], 0.0, ["Est%d" % d])
                    MS("gpsimd", Cb[d][0], 0.0, ["Cb%d0" % d])
                prev = [None, None]
                steps = [(i, d) for i in range(NCH) for d in range(2)]
                step0 = step

                def stepA(n):
                    i, d = steps[n]
                    c = order[d][i]
                    col = c * 8 + d * 4 + h
                    r4 = (step0 + n) % 4
                    vp, vpk = Vp[r4], "Vp%d" % r4
                    TS("vector", vp, Vx3[:, c, 0:257], Wt[:, col:col + 1], ALU.mult, ["Vx", "Vx1", "Wt"], [vpk])
                    if c >= 2:
                        for dh in range(2):
                            MM(ps[0][:, 0:128], KT3[:, dh, c * 128:(c + 1) * 128], QT3[:, dh, c * 128:(c + 1) * 128], dh == 0, dh == 1,
                               ["KT", "QT"], [PS[0]])
                        TT("vector", Smk[r4], ps[0][:, 0:128], maskb[:, d * 128:(d + 1) * 128], ALU.mult, [PS[0], "maskb"], ["Smk%d" % r4])

                def stepB(n):
                    i, d = steps[n]
                    c = order[d][i]
                    g = d * 4 + h
                    col = c * 8 + g
                    r4 = (step0 + n) % 4
                    cbk = "Cb%d%d" % (d, i % 2)
                    cbn = "Cb%d%d" % (d, (i + 1) % 2)
                    vp, vpk = Vp[r4], "Vp%d" % r4
                    lat = c >= 2
                    if lat:
                        smk, smkk = Smk[r4], "Smk%d" % r4
                        pn = ps[1 + d]
                        MM(pn[:, 0:257], smk, vp, True, False, [smkk, vpk], [PS[1 + d]])
                        for dh in range(2):
                            MM(pn[:, 0:257], QT3[:, dh, c * 128:(c + 1) * 128], Cb[d][i % 2][:, dh * 257:(dh + 1) * 257], False, dh == 1,
                               ["QT", cbk], [PS[1 + d]])
                    for dh in range(2):
                        pu = ps[3 + d * 2 + dh]
                        MM(pu[:, 0:257], Kt3[:, c, dh * 128:(dh + 1) * 128], vp, True, True, ["Ktok", vpk], [PS[3 + d * 2 + dh]])
                        pc = prev[d] if prev[d] is not None else col
                        STT("vector", Est[d][:, dh * 257:(dh + 1) * 257], Est[d][:, dh * 257:(dh + 1) * 257], DECb[:, pc:pc + 1],
                            pu[:, 0:257], ALU.mult, ALU.add, ["Est%d" % d, "DECb", PS[3 + d * 2 + dh]], ["Est%d" % d])
                    prev[d] = col
                    ACT(Cb[d][(i + 1) % 2], Est[d], AF.Copy, ["Est%d" % d, "DECb"], [cbn], scale=DECb[:, col:col + 1])
                    if lat:
                        s_ = sm[r4]
                        sk = "sm%d" % r4
                        TS("vector", s_[:, 3:4], pn[:, 256:257], -1.0, ALU.mult, [PS[1 + d]], [sk])
                        TT("vector", s_[:, 0:1], pn[:, 256:257], s_[:, 3:4], ALU.max, [PS[1 + d], sk], [sk])
                        TT("vector", s_[:, 1:2], s_[:, 0:1], EMTt[:, col:col + 1], ALU.max, [sk, "EMTt"], [sk])
                        RCP(s_[:, 2:3], s_[:, 1:2], [sk], [sk])
                        ACT(ho[r4], pn[:, 0:256], AF.Copy, [PS[1 + d], sk], ["ho%d" % r4], scale=s_[:, 2:3])
                        dst = (HF_d, HB_d)[d]
                        DMA("sync" if d == 0 else "scalar", dst[(c - 2) * 128:(c - 1) * 128, h * 256:(h + 1) * 256], ho[r4],
                            ["ho%d" % r4], ["H%d_d%d" % (d, i % 4)])

                stepA(0)
                for n in range(len(steps)):
                    if n + 1 < len(steps):
                        stepA(n + 1)
                    stepB(n)
                step += len(steps)
            A.release(m0)
            S.barrier()


        def cmul_store(pa, pa_k, pb_, pb_k, cr, ci, ck, sign, dst_re, dst_im, dre_k, dim_k, tmp, tk):
            t1, t2, t3, t4 = tmp
            TT("vector", t1, pa, cr, ALU.mult, [pa_k] + ck, [tk + "1"])
            TT("vector", t2, pb_, ci, ALU.mult, [pb_k] + ck, [tk + "2"])
            TT("vector", t3, pa, ci, ALU.mult, [pa_k] + ck, [tk + "3"])
            TT("vector", t4, pb_, cr, ALU.mult, [pb_k] + ck, [tk + "4"])
            if sign > 0:
                TT("gpsimd", dst_re, t1, t2, ALU.subtract, [tk + "1", tk + "2"], [dre_k])
                TT("vector", dst_im, t3, t4, ALU.add, [tk + "3", tk + "4"], [dim_k])
            else:
                TT("gpsimd", dst_re, t1, t2, ALU.add, [tk + "1", tk + "2"], [dre_k])
                TT("gpsimd", dst_im, t4, t3, ALU.subtract, [tk + "3", tk + "4"], [dim_k])

        def stageE():
            m0 = A.mark()
            PI = math.pi
            hid = A.alloc(NFFT, BF16)
            negt = A.alloc(128, F32)
            hfpar = A.alloc(4, F32)
            fb = A.alloc(2, F32)
            negpi = A.alloc(1, F32)
            w1t = A.alloc(64, F32)
            w2d = A.alloc(128, F32)
            DMA("sync", negt, c_negt[:, :], [], ["negt"])
            DMA("sync", hfpar, hfpar_d[:, :], [], ["hfpar"])
            DMA("sync", w1t[0:33, :], hf_w1[:, :], [], ["w1t"])
            DMA("sync", w2d[0:64, 0:64], hf_w2[:, :], [], ["w2d"])
            DMA("sync", w2d[0:64, 64:128], hf_w2[:, :], [], ["w2d"])
            MS("vector", negpi, -PI, ["negpi"])
            TT("vector", fb[:, 0:1], hfpar[:, 0:1], hfpar[:, 2:3], ALU.mult, ["hfpar"], ["fb"])
            TT("vector", fb[:, 1:2], hfpar[:, 1:2], hfpar[:, 2:3], ALU.mult, ["hfpar"], ["fb"])
            mm_ = A.mark()
            fch = [A.alloc(512, F32) for _ in range(2)]
            arg = [A.alloc(512, F32) for _ in range(2)]
            h1 = [A.alloc(512, F32) for _ in range(2)]
            ni = A.alloc(512, I32)
            nf = A.alloc(512, F32)
            mk = A.alloc(512, F32)

            def rr(x, xk, P_):
                TS("vector", x, x, 1.0 / (2 * PI), ALU.mult, [xk], [xk], s2=16.5, op1=ALU.add)
                CP("vector", ni[0:P_, :], x, [xk], ["rr_ni"])
                CP("vector", nf[0:P_, :], ni[0:P_, :], ["rr_ni"], ["rr_nf"])
                TT("vector", x, x, nf[0:P_, :], ALU.subtract, [xk, "rr_nf"], [xk])
                TS("vector", mk[0:P_, :], x, -1e12, ALU.mult, [xk], ["rr_mk"])
                TS("vector", mk[0:P_, :], mk[0:P_, :], 0.0, ALU.max, ["rr_mk"], ["rr_mk"], s2=1.0, op1=ALU.min)
                TT("vector", x, x, mk[0:P_, :], ALU.add, [xk, "rr_mk"], [xk])

            for q in range(32):
                i = q % 2
                DMA("sync", fch[i][0:33, :], c_featsT[:, q * 512:(q + 1) * 512], [], ["fch%d" % i])
                MM(ps[0][0:64, :], w1t[0:33, 0:64], fch[i][0:33, :], True, True, ["w1t", "fch%d" % i], [PS[0]])
                TS("vector", arg[i][0:64, :], ps[0][0:64, :], hfpar[0:64, 2:3], ALU.mult, [PS[0], "hfpar", "fb"], ["arg%d" % i],
                   s2=fb[0:64, 0:1], op1=ALU.add)
                rr(arg[i][0:64, :], "arg%d" % i, 64)
                ACT(h1[i][0:64, :], arg[i][0:64, :], AF.Sin, ["arg%d" % i, "negpi"], ["h1%d" % i], bias=negpi[0:64, 0:1], scale=2 * PI)
                MM(ps[1][:, :], w2d[0:64, :], h1[i][0:64, :], True, True, ["w2d", "h1%d" % i], [PS[1]])
                TS("vector", arg[i], ps[1][:, :], hfpar[:, 2:3], ALU.mult, [PS[1], "hfpar", "fb"], ["arg%d" % i], s2=fb[:, 1:2], op1=ALU.add)
                rr(arg[i], "arg%d" % i, 128)
                ACT(hid[:, q * 512:(q + 1) * 512], arg[i], AF.Sin, ["arg%d" % i, "negpi"], ["hid"], bias=negpi[:, 0:1], scale=2 * PI)
            MS("vector", hid[0:64, L:NFFT], 0.0, ["hid"])
            MS("vector", hid[64:128, 0:L + 1], 0.0, ["hid"])
            A.release(mm_)
            kbx = A.alloc(NFFT, BF16)
            xs = A.alloc(NFFT, BF16)
            w3s = A.alloc(128, F32)
            w3a = A.alloc(128, BF16)
            dbc = A.alloc(128, F32)
            hbc = A.alloc(128, F32)
            rnb = A.alloc(128, F32)
            ssr = A.alloc(512, F32, parts=1)
            win = [A.alloc(512, F32) for _ in range(2)]
            sq = [A.alloc(512, BF16) for _ in range(2)]
            tmpP = [[A.alloc(512, F32) for _ in range(4)] for _ in range(2)]
            tmp2P = [[A.alloc(512, F32) for _ in range(4)] for _ in range(2)]
            zbP = [[A.alloc(512, BF16) for _ in range(2)] for _ in range(2)]
            ob = [[A.alloc(512, BF16) for _ in range(2)] for _ in range(2)]
            ib = [[A.alloc(512, BF16) for _ in range(2)] for _ in range(2)]
            kb2 = [[A.alloc(512, BF16) for _ in range(2)] for _ in range(2)]
            hyo = [A.alloc(512, BF16) for _ in range(2)]
            tcv = lambda q: tw[:, 4 * q:4 * q + 4].unsqueeze(2).to_broadcast([128, 4, 128])
            tsv = lambda q: tw[:, 128 + 4 * q:128 + 4 * q + 4].unsqueeze(2).to_broadcast([128, 4, 128])
            p3 = lambda t_: v3(t_, 4, 128)
            Gk = lambda kind: ["G%d_w%d" % (kind, j) for j in range(4)]
            S_v = S_d.rearrange("(a b) c -> a b c", b=128)
            X0_v = X0_d.rearrange("(a b) c -> a b c", b=128)
            HY_v = HY_d.rearrange("(a b) c -> a b c", b=128)

            def fwd12(src, K, kind, skey):
                for q in range(32):
                    i = q % 2
                    a_, b_ = 2 * i, 2 * i + 1
                    MM(ps[a_][:, :], Cm[0:K, :], src[0:K, q * 512:(q + 1) * 512], True, True, ["dftb", skey], [PS[a_]])
                    MM(ps[b_][:, :], Sm_[0:K, :], src[0:K, q * 512:(q + 1) * 512], True, True, ["dftb", skey], [PS[b_]])
                    t1, t2, t3, t4 = tmpP[i]
                    tk = "tmp%d_" % i
                    TT("vector", p3(t1), p3(ps[a_][:, :]), tcv(q), ALU.mult, [PS[a_], "tw"], [tk + "1"])
                    TT("vector", p3(t2), p3(ps[b_][:, :]), tsv(q), ALU.mult, [PS[b_], "tw"], [tk + "2"])
                    TT("vector", p3(t3), p3(ps[b_][:, :]), tcv(q), ALU.mult, [PS[b_], "tw"], [tk + "3"])
                    TT("vector", p3(t4), p3(ps[a_][:, :]), tsv(q), ALU.mult, [PS[a_], "tw"], [tk + "4"])
                    TT("gpsimd", ob[i][0], t1, t2, ALU.subtract, [tk + "1", tk + "2"], ["ob%d0" % i])
                    STT("vector", ob[i][1], t3, -1.0, t4, ALU.mult, ALU.subtract, [tk + "3", tk + "4"], ["ob%d1" % i])
                    for comp in range(2):
                        DMA("scalar", G_d[kind, comp, :, 4 * q:4 * q + 4, :], p3(ob[i][comp]),
                            ["ob%d%d" % (i, comp)], ["G%d_w%d" % (kind, (2 * q + comp) % 4)])

            def fwd34_load(kind, q):
                i = q % 2
                for comp in range(2):
                    DMA("sync", p3(ib[i][comp]), G_d[kind, comp, 4 * q:4 * q + 4, :, :].rearrange("k b c -> b k c"),
                        Gk(kind), ["ib%d%d" % (i, comp)])

            def fwd34(kind, q, a_, b_):
                i = q % 2
                gre, gim = ib[i]
                MM(ps[a_][:, :], Cm, gre, True, False, ["dftb", "ib%d0" % i], [PS[a_]])
                MM(ps[a_][:, :], Sm_, gim, False, True, ["dftb", "ib%d1" % i], [PS[a_]])
                MM(ps[b_][:, :], Cm, gim, True, False, ["dftb", "ib%d1" % i], [PS[b_]])
                MM(ps[b_][:, :], nSm, gre, False, True, ["dftb", "ib%d0" % i], [PS[b_]])

            for cb in range(8):
                c0 = cb * 128
                DMA("sync", w3s[0:64, :], hf_w3[:, c0:c0 + 128], [], ["w3s"])
                DMA("sync", w3s[64:128, :], hf_w3[:, D + c0:D + c0 + 128], [], ["w3s"])
                CP("vector", w3a, w3s, ["w3s"], ["w3a"])
                DMA("sync", dbc, c_deltas[c0:c0 + 128].partition_broadcast(128), [], ["dbc"])
                DMA("sync", hbc, h_bias[c0:c0 + 128].partition_broadcast(128), [], ["hbc"])
                for q in range(32):
                    i = q % 2
                    pk = 5 - i
                    for bb in range(4):
                        b = 4 * q + bb
                        MM(ps[pk][:, bb * 128:(bb + 1) * 128], hid[:, b:NFFT:128], w3a, True, True, ["hid", "w3a"], [PS[pk]])
                        ACT(win[i][:, bb * 128:(bb + 1) * 128], dbc, AF.Exp, ["dbc", "negt"], ["win%d" % i], scale=negt[:, b:b + 1])
                    TT("vector", kbx[:, q * 512:(q + 1) * 512], ps[pk][:, :], win[i], ALU.mult, [PS[pk], "win%d" % i], ["kbx"])
                    TT("gpsimd", sq[i], kbx[:, q * 512:(q + 1) * 512], kbx[:, q * 512:(q + 1) * 512], ALU.mult, ["kbx"], ["sq%d" % i])
                    MM(ps[6][0:1, :], onesb[:, 0:1], sq[i], q == 0, q == 31, ["onesb", "sq%d" % i], [PS[6]])
                if "KERN_d" in dbg and cb == 0:
                    DMA("sync", KERN_d[:, :, :], v3(kbx, 128, 128), ["kbx"], ["KERN_d"])
                CP("vector", ssr[0:1, :], ps[6][0:1, :], [PS[6]], ["ssr"])
                TT("vector", ssr[0:1, 0:256], ssr[0:1, 0:256], ssr[0:1, 256:512], ALU.add, ["ssr"], ["ssr"])
                TT("vector", ssr[0:1, 0:128], ssr[0:1, 0:128], ssr[0:1, 128:256], ALU.add, ["ssr"], ["ssr"])
                ACT(ssr[0:1, 128:256], ssr[0:1, 0:128], AF.Sqrt, ["ssr", "epst"], ["ssr"], bias=epst[0:1, 0:1])
                RCP(ssr[0:1, 256:384], ssr[0:1, 128:256], ["ssr"], ["ssr"])
                TS("vector", ssr[0:1, 256:384], ssr[0:1, 256:384], 1.0 / NFFT, ALU.mult, ["ssr"], ["ssr"])
                MM(ps[6][0:64, 0:128], ones32[0:1, 0:64], ssr[0:1, 256:384], True, True, ["ones32", "ssr"], [PS[6]])
                CP("vector", rnb[0:64, :], ps[6][0:64, 0:128], [PS[6]], ["rnb"])
                DMA("sync", v3(xs[0:64, :], 128, 128), S_v[:, :, c0:c0 + 128], ["S_d%d" % j for j in range(4)], ["xs"])
                fwd12(kbx, 128, 1, "kbx")
                fwd12(xs, 64, 0, "xs")
                fwd34_load(1, 0)
                for q in range(32):
                    i = q % 2
                    if q + 1 < 32:
                        fwd34_load(1, q + 1)
                    fwd34(1, q, 2 * i, 2 * i + 1)
                    CP("scalar", kb2[i][0], ps[2 * i][:, :], [PS[2 * i]], ["kb2%d0" % i])
                    CP("scalar", kb2[i][1], ps[2 * i + 1][:, :], [PS[2 * i + 1]], ["kb2%d1" % i])
                    for comp in range(2):
                        DMA("scalar", KH_d[comp, :, 4 * q:4 * q + 4, :], p3(kb2[i][comp]), ["kb2%d%d" % (i, comp)],
                            ["KH_w%d" % ((2 * q + comp) % 4)])
                DMA("sync", v3(kbx[0:64, :], 128, 128), X0_v[:, :, c0:c0 + 128], ["X0_d%d" % j for j in range(4)], ["kbx"])
                def sig_load(q):
                    fwd34_load(0, q)
                    for comp in range(2):
                        DMA("sync", p3(kb2[q % 2][comp]), KH_d[comp, :, 4 * q:4 * q + 4, :],
                            ["KH_w%d" % j for j in range(4)], ["kb2%d%d" % (q % 2, comp)])

                sig_load(0)
                for q in range(32):
                    i = q % 2
                    if q + 1 < 32:
                        sig_load(q + 1)
                    xa_, xb_ = (0, 1) if i == 0 else (4, 5)
                    fwd34(0, q, xa_, xb_)
                    zb = zbP[i]
                    tmp = tmpP[i]
                    tmp2 = tmp2P[i]
                    cmul_store(ps[xa_][:, :], PS[xa_], ps[xb_][:, :], PS[xb_], kb2[i][0], kb2[i][1], ["kb2%d0" % i, "kb2%d1" % i], +1,
                               zb[0], zb[1], "zb%d0" % i, "zb%d1" % i, tmp, "tmp%d_" % i)
                    MM(ps[2][:, :], Cm, zb[0], True, False, ["dftb", "zb%d0" % i], [PS[2]])
                    MM(ps[2][:, :], nSm, zb[1], False, True, ["dftb", "zb%d1" % i], [PS[2]])
                    MM(ps[3][:, :], Cm, zb[1], True, False, ["dftb", "zb%d1" % i], [PS[3]])
                    MM(ps[3][:, :], Sm_, zb[0], False, True, ["dftb", "zb%d0" % i], [PS[3]])
                    t1, t2, t3, t4 = tmp2
                    qk_ = "tmq%d_" % i
                    TT("vector", p3(t1), p3(ps[2][:, :]), tcv(q), ALU.mult, [PS[2], "tw"], [qk_ + "1"])
                    TT("vector", p3(t2), p3(ps[3][:, :]), tsv(q), ALU.mult, [PS[3], "tw"], [qk_ + "2"])
                    TT("vector", p3(t3), p3(ps[2][:, :]), tsv(q), ALU.mult, [PS[2], "tw"], [qk_ + "3"])
                    TT("vector", p3(t4), p3(ps[3][:, :]), tcv(q), ALU.mult, [PS[3], "tw"], [qk_ + "4"])
                    TT("gpsimd", ob[i][0], t1, t2, ALU.subtract, [qk_ + "1", qk_ + "2"], ["ob%d0" % i])
                    TT("vector", ob[i][1], t3, t4, ALU.add, [qk_ + "3", qk_ + "4"], ["ob%d1" % i])
                    for comp in range(2):
                        DMA("scalar", H_d[comp, :, 4 * q:4 * q + 4, :], p3(ob[i][comp]), ["ob%d%d" % (i, comp)],
                            ["H_w%d" % ((2 * q + comp) % 4)])
                def fin_load(q):
                    for comp in range(2):
                        DMA("sync", p3(ib[q % 2][comp]), H_d[comp, 4 * q:4 * q + 4, :, :].rearrange("b k c -> k b c"),
                            ["H_w%d" % j for j in range(4)], ["ib%d%d" % (q % 2, comp)])

                fin_load(0)
                for q in range(32):
                    i = q % 2
                    if q + 1 < 32:
                        fin_load(q + 1)
                    pe_ = 4 + i
                    MM(ps[pe_][0:64, :], Cm[:, 0:64], ib[i][0], True, False, ["dftb", "ib%d0" % i], [PS[pe_]])
                    MM(ps[pe_][0:64, :], nSm[:, 0:64], ib[i][1], False, True, ["dftb", "ib%d1" % i], [PS[pe_]])
                    t1, t2 = tmpP[i][0], tmpP[i][1]
                    k1_, k2_ = "tmp%d_1" % i, "tmp%d_2" % i
                    TT("vector", p3(t1[0:64, :]), p3(ps[pe_][0:64, :]), rnb[0:64, :].unsqueeze(1).to_broadcast([64, 4, 128]), ALU.mult,
                       [PS[pe_], "rnb"], [k1_])
                    TT("gpsimd", p3(t2[0:64, :]), p3(xs[0:64, q * 512:(q + 1) * 512]), hbc[0:64, :].unsqueeze(1).to_broadcast([64, 4, 128]),
                       ALU.mult, ["xs", "hbc"], [k2_])
                    TT("gpsimd", t1[0:64, :], t1[0:64, :], t2[0:64, :], ALU.add, [k1_, k2_], [k1_])
                    TT("vector", hyo[i][0:64, :], t1[0:64, :], kbx[0:64, q * 512:(q + 1) * 512], ALU.mult, [k1_, "kbx"], ["hyo%d" % i])
                    DMA("scalar", HY_v[:, 4 * q:4 * q + 4, c0:c0 + 128], p3(hyo[i][0:64, :]), ["hyo%d" % i], ["HY_d%d" % (q % 4)])
            A.release(m0)
            S.barrier()

        def transpose8(src_b, skey, dst_b, dkey):
            for k in range(8):
                TR(pb[:, k * 128:(k + 1) * 128], src_b[:, k * 128:(k + 1) * 128], identb, [skey, "identb"], ["pb"])
            CP("scalar", dst_b, pb[:, :], ["pb"], [dkey])

        def stageF1():
            m = A.mark()
            norm_setup()
            stage = [A.alloc(8 * 512, F32) for _ in range(2)]
            wph = A.alloc(8 * 3072, BF16)
            wph3 = v3(wph, 8, 3072)
            bph = A.alloc(3072, F32)
            for part, c_ in enumerate((O0, GA0, GB0)):
                DMA("sync", bph[:, part * D:(part + 1) * D], b_in[c_:c_ + D].partition_broadcast(128), [], ["bph"])
                for g in range(2):
                    gg = part * 2 + g
                    load_w_bf16(wph3[:, :, gg * 512:(gg + 1) * 512], "wph", c_ + g * 512, 512, stage[gg % 2], "stage%d" % (gg % 2),
                                "sync" if gg % 2 == 0 else "scalar")
            hb = [A.alloc(D, BF16) for _ in range(2)]
            hT = [A.alloc(D, BF16) for _ in range(2)]
            tf = [A.alloc(512, F32) for _ in range(2)]
            og = [A.alloc(3072, BF16) for _ in range(2)]
            for tt in range(16):
                i = tt % 2
                norm_tile(xq[tt * 128:(tt + 1) * 128, :], G1, "G1", SH1, "SH1", hb[i], "fhb%d" % i)
                transpose8(hb[i], "fhb%d" % i, hT[i], "fhT%d" % i)
                h3 = v3(hT[i], 8, 128)
                for gg in range(6):
                    pi = gg % 2
                    for k in range(8):
                        MM(ps[pi][:, :], h3[:, k, :], wph3[:, k, gg * 512:(gg + 1) * 512], k == 0, k == 7, ["fhT%d" % i, "wph"], [PS[pi]])
                    TT("vector", tf[pi], ps[pi][:, :], bph[:, gg * 512:(gg + 1) * 512], ALU.add, [PS[pi], "bph"], ["tf%d" % pi])
                    ACT(og[i][:, gg * 512:(gg + 1) * 512], tf[pi], AF.Sigmoid, ["tf%d" % pi], ["og%d" % i])
                DMA("sync", OG_d[tt * 128:(tt + 1) * 128, :], og[i], ["og%d" % i], ["OG_d%d" % i])
            A.release(m)
            S.barrier()

        ngath = [0]

        def gather(dst, dkey, src2d, idxcol, nrows, rkeys):
            ngath[0] += 1
            S.dma("gpsimd", lambda e: e.indirect_dma_start(out=dst, out_offset=None, in_=src2d,
                                                           in_offset=bass.IndirectOffsetOnAxis(ap=idxcol, axis=0),
                                                           bounds_check=nrows - 1, oob_is_err=False), rkeys, [dkey, "indslot%d" % (ngath[0] % 2)])

        def stageF2():
            m = A.mark()
            norm_setup(1, with_x=False)
            stage = [A.alloc(8 * 512, F32) for _ in range(2)]
            wabo = [A.alloc(8 * D, BF16) for _ in range(3)]
            for wi, wsrc in enumerate((w_a, w_b, w_out)):
                wv_ = wsrc.rearrange("(k p) n -> p k n", p=128)
                for g in range(2):
                    st_ = stage[(wi * 2 + g) % 2]
                    sk = "stage%d" % ((wi * 2 + g) % 2)
                    DMA("sync" if g == 0 else "scalar", v3(st_, 8, 512), wv_[:, :, g * 512:(g + 1) * 512], [], [sk])
                    CP("gpsimd", v3(wabo[wi], 8, D)[:, :, g * 512:(g + 1) * 512], v3(st_, 8, 512), [sk], ["wabo%d" % wi])
            wr32 = A.alloc(8 * 72, F32)
            brt = A.alloc(72, F32)
            idx = st.enter_context(nc.sbuf_tensor("idxt", [128, 16], I32))
            DMA("sync", v3(wr32, 8, 72), w_rt.rearrange("(k p) n -> p k n", p=128), [], ["wr32"])
            DMA("sync", brt, b_rt.partition_broadcast(128), [], ["brt"])
            DMA("sync", idx[:, :], idxq[:, :], [], ["idx"])
            og = [A.alloc(3072, BF16) for _ in range(2)]
            gthc = [A.alloc(3 * D, BF16) for _ in range(2)]
            gth = [[g_[:, 0:D], g_[:, D:2 * D], g_[:, 2 * D:3 * D]] for g_ in gthc]
            ab = A.alloc(D, BF16)
            aT = A.alloc(D, BF16)
            hyT = A.alloc(D, BF16)
            m1 = A.alloc(D, F32)
            m2 = A.alloc(512, F32)
            mb = A.alloc(D, BF16)
            mT = A.alloc(D, BF16)
            xres = A.alloc(D, F32)
            x1t = A.alloc(D, F32)
            h2f = A.alloc(D, F32)
            h2b = A.alloc(D, BF16)
            h2T = A.alloc(D, BF16)
            h2T32 = A.alloc(D, F32)
            wg_all = A.alloc(16 * 64, F32)
            rt = A.alloc(256, F32)
            wa3, wb3, wo3 = (v3(w_, 8, D) for w_ in wabo)
            for tt in range(16):
                i = tt % 2
                DMA("sync", og[i], OG_d[tt * 128:(tt + 1) * 128, :], ["OG_d0", "OG_d1"], ["fog%d" % i])
                DMA("sync", xres, xq[tt * 128:(tt + 1) * 128, :], [], ["xres"])
                hk_ = ["H0_d%d" % j for j in range(4)] + ["H1_d%d" % j for j in range(4)]
                gather(gthc[i], "gth%d" % i, HC_d[:, :], idx[:, tt:tt + 1], L, ["idx"] + hk_ + ["HY_d%d" % j for j in range(4)])
                TT("gpsimd", m1, gth[i][0], gth[i][1], ALU.add, ["gth%d" % i], ["m1"])
                TT("vector", ab, m1, og[i][:, 0:D], ALU.mult, ["m1", "fog%d" % i], ["ab"])
                transpose8(ab, "ab", aT, "aT")
                transpose8(gth[i][2], "gth%d" % i, hyT, "hyT")
                a3, y3 = v3(aT, 8, 128), v3(hyT, 8, 128)
                for half in range(2):
                    for k in range(8):
                        MM(ps[0][:, :], a3[:, k, :], wa3[:, k, half * 512:(half + 1) * 512], k == 0, k == 7, ["aT", "wabo0"], [PS[0]])
                    for k in range(8):
                        MM(ps[1][:, :], y3[:, k, :], wb3[:, k, half * 512:(half + 1) * 512], k == 0, k == 7, ["hyT", "wabo1"], [PS[1]])
                    TT("vector", m1[:, half * 512:(half + 1) * 512], ps[0][:, :], og[i][:, D + half * 512:D + (half + 1) * 512], ALU.mult,
                       [PS[0], "fog%d" % i], ["m1"])
                    TT("vector", m2, ps[1][:, :], og[i][:, 2 * D + half * 512:2 * D + (half + 1) * 512], ALU.mult, [PS[1], "fog%d" % i], ["m2"])
                    TT("gpsimd", mb[:, half * 512:(half + 1) * 512], m1[:, half * 512:(half + 1) * 512], m2, ALU.add, ["m1", "m2"], ["mb"])
                transpose8(mb, "mb", mT, "mT")
                mT3 = v3(mT, 8, 128)
                for half in range(2):
                    for k in range(8):
                        MM(ps[2][:, :], mT3[:, k, :], wo3[:, k, half * 512:(half + 1) * 512], k == 0, k == 7, ["mT", "wabo2"], [PS[2]])
                    TT("vector", m1[:, half * 512:(half + 1) * 512], ps[2][:, :], GT1[:, half * 512:(half + 1) * 512], ALU.mult,
                       [PS[2], "GT1"], ["m1"])
                TT("gpsimd", x1t, m1, xres, ALU.add, ["m1", "xres"], ["x1t"])
                DMA("sync", X1_d[tt * 128:(tt + 1) * 128, :], x1t, ["x1t"], ["X1_d%d" % i])
                norm_tile(None, G2, "G2", SH2, "SH2", h2f, "h2f", x_keep=(x1t, "x1t"))
                CP("vector", h2b, h2f, ["h2f"], ["h2b"])
                transpose8(h2b, "h2b", h2T, "h2T")
                DMA("sync", H2T_d[:, :, tt * 128:(tt + 1) * 128], v3(h2T, 8, 128), ["h2T"], ["H2T_d%d" % i])
                for k in range(8):
                    TR(ps[3 + k // 4][:, (k % 4) * 128:(k % 4 + 1) * 128], h2f[:, k * 128:(k + 1) * 128], ident32, ["h2f", "ident32"],
                       [PS[3 + k // 4]])
                for hh in range(2):
                    CP("scalar", h2T32[:, hh * 512:(hh + 1) * 512], ps[3 + hh][:, :], [PS[3 + hh]], ["h2T32"])
                for k in range(8):
                    MM(ps[5][:, 0:72], h2T32[:, k * 128:(k + 1) * 128], v3(wr32, 8, 72)[:, k, :], k == 0, k == 7, ["h2T32", "wr32"], [PS[5]])
                lg = rt[:, 0:72]
                gmax, negm, se, gw = rt[:, 72:73], rt[:, 73:74], rt[:, 74:75], rt[:, 75:76]
                ohg = rt[:, 80:88]
                eg = rt[:, 88:96]
                els = rt[:, 96:104]
                oh1 = rt[:, 104:112]
                els2 = rt[:, 112:120]
                oh2 = rt[:, 120:128]
                m1_, m2_, dm, ed = rt[:, 128:129], rt[:, 129:130], rt[:, 130:131], rt[:, 131:132]
                w1_, w2_ = rt[:, 132:133], rt[:, 133:134]
                wv = rt[:, 136:144]
                t64 = rt[:, 144:208]
                R_ = ["rt"]
                TT("vector", lg, ps[5][:, 0:72], brt, ALU.add, [PS[5], "brt"], R_)
                RED("vector", gmax, rt[:, 0:8], ALU.max, R_, R_)
                TS("vector", ohg, rt[:, 0:8], gmax, ALU.subtract, R_, R_)
                TS("vector", ohg, ohg, 1e30, ALU.mult, R_, R_, s2=1.0, op1=ALU.add)
                TS("vector", ohg, ohg, 0.0, ALU.max, R_, R_, s2=1.0, op1=ALU.min)
                TS("vector", negm, gmax, -1.0, ALU.mult, R_, R_)
                MS("vector", se, 0.0, R_)
                ACT(eg, rt[:, 0:8], AF.Exp, R_, R_, bias=negm, accum=se)
                RCP(gw, se, R_, R_)
                TT("vector", v3(t64, 8, 8), v3(rt[:, 8:72], 8, 8), ohg.unsqueeze(2).to_broadcast([128, 8, 8]), ALU.mult, R_, R_)
                RED("vector", els, v3(t64, 8, 8).rearrange("p g e -> p e g"), ALU.add, R_, R_)
                RED("vector", m1_, els, ALU.max, R_, R_)
                TS("vector", oh1, els, m1_, ALU.subtract, R_, R_)
                TS("vector", oh1, oh1, 1e30, ALU.mult, R_, R_, s2=1.0, op1=ALU.add)
                TS("vector", oh1, oh1, 0.0, ALU.max, R_, R_, s2=1.0, op1=ALU.min)
                STT("vector", els2, oh1, -1e30, els, ALU.mult, ALU.add, R_, R_)
                RED("vector", m2_, els2, ALU.max, R_, R_)
                TS("vector", oh2, els2, m2_, ALU.subtract, R_, R_)
                TS("vector", oh2, oh2, 1e30, ALU.mult, R_, R_, s2=1.0, op1=ALU.add)
                TS("vector", oh2, oh2, 0.0, ALU.max, R_, R_, s2=1.0, op1=ALU.min)
                TT("vector", dm, m2_, m1_, ALU.subtract, R_, R_)
                ACT(ed, dm, AF.Exp, R_, R_)
                TS("vector", m1_, ed, 1.0, ALU.add, R_, R_)
                RCP(w1_, m1_, R_, R_)
                TT("vector", w2_, ed, w1_, ALU.mult, R_, R_)
                TT("vector", w1_, w1_, gw, ALU.mult, R_, R_)
                TT("vector", w2_, w2_, gw, ALU.mult, R_, R_)
                TS("vector", wv, oh1, w1_, ALU.mult, R_, R_)
                STT("vector", wv, oh2, w2_, wv, ALU.mult, ALU.add, R_, R_)
                TT("vector", v3(wg_all[:, tt * 64:(tt + 1) * 64], 8, 8), ohg.unsqueeze(2).to_broadcast([128, 8, 8]),
                   wv.unsqueeze(1).to_broadcast([128, 8, 8]), ALU.mult, R_, ["wg_all"])
            DMA("sync", WGT_d[:, :], wg_all, ["wg_all"], ["WGT_d"])
            A.release(m)
            S.barrier()

        def stageF3():
            A.release(mark_low)
            m = A.mark()
            h2T = A.alloc(8 * NQ, BF16)
            acc = A.alloc(16 * D, F32)
            wgt = A.alloc(16 * 64, F32)
            wex = [A.alloc(12 * 1024, BF16) for _ in range(2)]
            stage = [A.alloc(4 * 512, F32) for _ in range(2)]
            actT = [A.alloc(4 * 512, BF16) for _ in range(2)]
            s1 = [A.alloc(512, F32) for _ in range(2)]
            h3 = v3(h2T, 8, NQ)
            DMA("sync", h3, H2T_d[:, :, :], ["H2T_d0", "H2T_d1"], ["h2Tm"])
            DMA("sync", wgt, WGT_d[:, :], ["WGT_d"], ["wgt"])
            MS("vector", acc, 0.0, ["acc"])
            w1v = w1_e.rearrange("e (k p) n -> e p k n", p=128)
            w3v = w3_e.rearrange("e (k p) n -> e p k n", p=128)
            w2v = w2_e.rearrange("e (k p) n -> e p k n", p=128)
            pc = [0]

            def load_expert(e):
                wb_ = wex[e % 2]
                wk = "wex%d" % (e % 2)
                w1b, w3b, w2b = v3(wb_[:, 0:4096], 8, 512), v3(wb_[:, 4096:8192], 8, 512), v3(wb_[:, 8192:12288], 4, 1024)
                pieces = []
                for hh in range(2):
                    pieces.append((w1b[:, hh * 4:(hh + 1) * 4, :], w1v[e, :, hh * 4:(hh + 1) * 4, :], 4, 512))
                for hh in range(2):
                    pieces.append((w3b[:, hh * 4:(hh + 1) * 4, :], w3v[e, :, hh * 4:(hh + 1) * 4, :], 4, 512))
                for hh in range(2):
                    pieces.append((w2b[:, hh * 2:(hh + 1) * 2, :], w2v[e, :, hh * 2:(hh + 1) * 2, :], 2, 1024))
                for dstv, srcv, a_, b_ in pieces:
                    sb2 = stage[pc[0] % 2]
                    sk = "mstage%d" % (pc[0] % 2)
                    DMA("sync" if pc[0] % 2 == 0 else "scalar", v3(sb2, a_, b_), srcv, [], [sk])
                    CP("gpsimd", dstv, v3(sb2, a_, b_), [sk], [wk])
                    pc[0] += 1

            def wviews(e):
                wb_ = wex[e % 2]
                return v3(wb_[:, 0:4096], 8, 512), v3(wb_[:, 4096:8192], 8, 512), v3(wb_[:, 8192:12288], 4, 1024), "wex%d" % (e % 2)

            def h_phase(it):
                e, tb = divmod(it, 4)
                w1b, w3b, w2b, wk = wviews(e)
                at = actT[it % 2]
                ak = "actT%d" % (it % 2)
                for ft in range(4):
                    pa, pb2 = (0, 1) if ft % 2 == 0 else (2, 3)
                    for k in range(8):
                        MM(ps[pa][:, :], w1b[:, k, ft * 128:(ft + 1) * 128], h3[:, k, tb * 512:(tb + 1) * 512], k == 0, k == 7, [wk, "h2Tm"],
                           [PS[pa]])
                    for k in range(8):
                        MM(ps[pb2][:, :], w3b[:, k, ft * 128:(ft + 1) * 128], h3[:, k, tb * 512:(tb + 1) * 512], k == 0, k == 7, [wk, "h2Tm"],
                           [PS[pb2]])
                    ACT(s1[ft % 2], ps[pa][:, :], AF.Silu, [PS[pa]], ["s1%d" % (ft % 2)])
                    TT("vector", at[:, ft * 512:(ft + 1) * 512], s1[ft % 2], ps[pb2][:, :], ALU.mult, ["s1%d" % (ft % 2), PS[pb2]], [ak])

            def y_phase(it):
                e, tb = divmod(it, 4)
                w1b, w3b, w2b, wk = wviews(e)
                at = actT[it % 2]
                ak = "actT%d" % (it % 2)
                for t4 in range(4):
                    tile_ = tb * 4 + t4
                    for half in range(2):
                        py = 4 + half
                        for ft in range(4):
                            MM(ps[py][:, :], at[:, ft * 512 + t4 * 128:ft * 512 + (t4 + 1) * 128], w2b[:, ft, half * 512:(half + 1) * 512],
                               ft == 0, ft == 3, [ak, wk], [PS[py]])
                        asl = acc[:, tile_ * D + half * 512:tile_ * D + (half + 1) * 512]
                        STT("vector", asl, ps[py][:, :], wgt[:, tile_ * 64 + e:tile_ * 64 + e + 1], asl, ALU.mult, ALU.add,
                            [PS[py], "wgt", "acc"], ["acc"])

            load_expert(0)
            h_phase(0)
            for it in range(256):
                e, tb = divmod(it, 4)
                if tb == 0 and e + 1 < 64:
                    load_expert(e + 1)
                if it + 1 < 256:
                    h_phase(it + 1)
                y_phase(it)
            gfin = A.alloc(D, F32)
            xt = [A.alloc(D, F32) for _ in range(2)]
            junk = A.alloc(D, BF16)
            st4 = A.alloc(8, F32)
            DMA("sync", gfin, g_fin.partition_broadcast(128), [], ["gfin"])
            if "MOE_d" in dbg:
                DMA("sync", MOE_d.rearrange("(t p) d -> p t d", p=128), v3(acc, 16, D), ["acc"], ["MOE_d"])
            for tt in range(16):
                i = tt % 2
                x_ = xt[i]
                xk = "fx%d" % i
                DMA("sync", x_, X1_d[tt * 128:(tt + 1) * 128, :], ["X1_d0", "X1_d1"], [xk])
                asl = acc[:, tt * D:(tt + 1) * D]
                TT("vector", asl, asl, GT2, ALU.mult, ["acc", "GT2"], ["acc"])
                TT("gpsimd", x_, x_, asl, ALU.add, [xk, "acc"], [xk])
                MS("vector", st4[:, 0:1], 0.0, ["st4"])
                ACT(junk, x_, AF.Square, [xk, "st4"], ["fjunk", "st4"], accum=st4[:, 0:1])
                ACT(st4[:, 1:2], st4[:, 0:1], AF.Sqrt, ["st4", "epst"], ["st4"], bias=epst[:, 0:1], scale=1.0 / D)
                RCP(st4[:, 2:3], st4[:, 1:2], ["st4"], ["st4"])
                STT("vector", x_, x_, st4[:, 2:3], gfin, ALU.mult, ALU.mult, [xk, "st4", "gfin"], [xk])
                DMA("sync", out[tt * 128:(tt + 1) * 128, :], x_, [xk], ["out%d" % i])
            A.release(m)

        G1c, SH1c = stageA()
        if upto == "A":
            S.emit(st)
            return nc
        stageB(G1c, SH1c)
        if upto == "B":
            S.emit(st)
            return nc
        if "C1" not in skip:
            stageC1()
        if upto == "C1":
            S.emit(st)
            return nc
        if "D" not in skip:
            stageD()
        if upto == "D":
            S.emit(st)
            return nc
        stageC2()
        if upto == "C2":
            S.emit(st)
            return nc
        stageE()
        if upto == "E":
            S.emit(st)
            return nc
        stageF1()
        stageF2()
        stageF3()
        S.emit(st)
        nc._sched = S
        nc._arena_peak = A.peak
    return nc


def make_in_maps(inputs):
    f = lambda a: np.ascontiguousarray(np.asarray(a, dtype=np.float32))
    x, c, ctx, c_ctx = f(inputs["x"]), f(inputs["c"]), f(inputs["ctx"]), f(inputs["c_ctx"])
    w_in, b_in = f(inputs["w_in"])[0], f(inputs["b_in"])[0]
    wqk, bqk = f(inputs["w_qk_conv"])[0], f(inputs["b_qk_conv"])[0]
    whc, bhc = f(inputs["w_h_conv"])[0], f(inputs["b_h_conv"])[0]
    qkpar = np.stack([wqk[0], wqk[1], wqk[2], bqk, b_in[0:2048]], axis=-1).reshape(16, 128, 5).transpose(1, 0, 2)
    hypar = np.stack([whc[0], whc[1], whc[2], bhc, b_in[HY0:GA0]], axis=-1).reshape(24, 128, 5).transpose(1, 0, 2)
    gpar = np.zeros((36, 2), np.float32)
    gpar[0:4, 0] = b_in[IG0:IG0 + 4]
    gpar[32:36, 0] = b_in[IG0 + 4:IG0 + 8]
    gpar[0:4, 1] = b_in[FG0:FG0 + 4]
    gpar[32:36, 1] = b_in[FG0 + 4:FG0 + 8]
    hfpar = np.zeros((128, 4), np.float32)
    for r in range(2):
        hfpar[r * 64:(r + 1) * 64, 0] = f(inputs["hf_b1"])[0]
        hfpar[r * 64:(r + 1) * 64, 1] = f(inputs["hf_b2"])[0]
        hfpar[r * 64:(r + 1) * 64, 2] = f(inputs["hf_freq"])[0]
    w_rt = np.concatenate([f(inputs["w_group"])[0], f(inputs["w_router"])[0]], axis=1)
    b_rt = np.concatenate([f(inputs["b_group"])[0], f(inputs["b_router"])[0]], axis=0)
    shared = {
        "w_mod": f(inputs["w_mod"])[0], "b_mod": f(inputs["b_mod"]), "g1": f(inputs["g_norm1"])[0], "g2": f(inputs["g_norm2"])[0],
        "w_in": w_in, "b_in": b_in, "qkpar": np.ascontiguousarray(qkpar.reshape(128, 80)),
        "hypar": np.ascontiguousarray(hypar.reshape(128, 120)), "gpar": gpar,
        "hf_w1": f(inputs["hf_w1"])[0], "hfpar": hfpar, "hf_w2": f(inputs["hf_w2"])[0], "hf_w3": f(inputs["hf_w3"])[0],
        "h_bias": f(inputs["h_bias"])[0], "w_a": f(inputs["w_a"])[0], "w_b": f(inputs["w_b"])[0], "w_out": f(inputs["w_out"])[0],
        "w_rt": np.ascontiguousarray(w_rt), "b_rt": np.ascontiguousarray(b_rt),
        "w1_e": f(inputs["w1_e"])[0], "w3_e": f(inputs["w3_e"])[0], "w2_e": f(inputs["w2_e"])[0], "g_fin": f(inputs["g_final"]),
    }
    shared.update(host_consts())
    maps = []
    for i in range(8):
        b, q = i // 4, i % 4
        cT = np.stack([c[b], c_ctx], axis=-1).reshape(8, 128, 2).transpose(1, 0, 2).reshape(128, 16)
        idx = (q * NQ + np.arange(NQ, dtype=np.int32)).reshape(16, 128).T
        m = dict(shared)
        m.update({"xb": x[b], "ctxb": ctx[b], "xq": np.ascontiguousarray(x[b, q * NQ:(q + 1) * NQ]),
                  "idxq": np.ascontiguousarray(idx.astype(np.int32)), "cT": np.ascontiguousarray(cT)})
        maps.append(m)
    return maps


_NC_CACHE = {}


def kernel(**inputs):
    maps = make_in_maps(inputs)
    if "nc" not in _NC_CACHE:
        _NC_CACHE["nc"] = build_nc()
    nc = _NC_CACHE["nc"]
    maps = [{k: m[k] for k in nc._in_names} for m in maps]
    res = run_bass_kernel_spmd(nc, maps, core_ids=list(range(8)))
    outp = np.zeros((2, L, D), np.float32)
    for i in range(8):
        b, q = i // 4, i % 4
        outp[b, q * NQ:(q + 1) * NQ] = np.asarray(res.results[i]["out"], dtype=np.float32)
    return outp
```

```python
import contextlib
import math

import numpy as np
import ml_dtypes

import concourse.bass as bass
import concourse.mybir as mybir
from concourse.bass_utils import run_bass_kernel_spmd

F32 = mybir.dt.float32
BF16 = mybir.dt.bfloat16
I32 = mybir.dt.int32
U8 = mybir.dt.uint8
AF = mybir.ActivationFunctionType
ALU = mybir.AluOpType
AX = mybir.AxisListType

D = 1024
L = 8192
CTX = 256
T = L + CTX
NCH = T // 128
NQ = 2048
IN_COLS = 9232
V0, O0, IG0, FG0, HY0, GA0, GB0 = 2048, 3072, 4096, 4104, 4112, 7184, 8208
NFFT = 2 * L
EPS = 1e-6


class Sched:
    ENGS = ("sync", "scalar", "vector", "gpsimd", "tensor")

    def __init__(self, nc):
        self.nc = nc
        self.ops = []
        self.last_w = {}
        self.readers = {}
        self.extra = {e: set() for e in self.ENGS}
        self.last_on = {}
        self.dmas_since_barrier = []
        self.barrier_at = []

    def _add(self, eng, fn, reads, writes, dma):
        i = len(self.ops)
        deps = set(self.extra[eng])
        self.extra[eng] = set()
        for k in list(reads) + list(writes):
            if k in self.last_w:
                deps.add(self.last_w[k])
        for k in writes:
            rd = self.readers.get(k)
            if rd:
                deps.update(rd[0].values())
                deps.update(rd[1])
        deps.discard(i)
        for k in reads:
            rd = self.readers.setdefault(k, ({}, []))
            if dma:
                rd[1].append(i)
            else:
                rd[0][eng] = i
        for k in writes:
            self.last_w[k] = i
            self.readers[k] = ({}, [])
        self.ops.append(dict(eng=eng, fn=fn, deps=deps, dma=dma,
                             key=(writes[0] if (dma and writes) else None)))
        self.last_on[eng] = i
        if dma:
            self.dmas_since_barrier.append(i)
        return i

    def op(self, eng, fn, reads=(), writes=()):
        return self._add(eng, fn, tuple(reads), tuple(writes), False)

    def dma(self, eng, fn, reads=(), writes=()):
        assert len(writes) >= 1
        return self._add(eng, fn, tuple(reads), tuple(writes), True)

    def barrier(self):
        self.barrier_at.append(len(self.ops))
        deps = set(self.last_on.values()) | set(self.dmas_since_barrier)
        self.dmas_since_barrier = []
        for e in self.ENGS:
            self.extra[e] |= deps

    def emit(self, stack, final_wait_eng="sync"):
        nc = self.nc
        ops = self.ops
        self.barrier()
        self.op(final_wait_eng, None)
        need = set()
        for o in ops:
            for d in o["deps"]:
                od = ops[d]
                if od["dma"]:
                    continue
                if od["eng"] == "tensor" and o["eng"] == "tensor" and not o["dma"]:
                    continue
                need.add(d)
        esem = {e: stack.enter_context(nc.semaphore("se_" + e)) for e in self.ENGS}
        ksem, kcnt, sig = {}, {}, {}
        cnt = {e: 0 for e in self.ENGS}
        pool, allsems = [], []
        bset = set(self.barrier_at)
        maxk = 0
        for i, o in enumerate(ops):
            if i in bset:
                pool.extend(ksem.values())
                ksem = {}
            if o["dma"]:
                k = o["key"]
                if k not in ksem:
                    if pool:
                        ksem[k] = pool.pop()
                    else:
                        sm_ = stack.enter_context(nc.semaphore("sd_%d" % len(allsems)))
                        allsems.append(sm_)
                        kcnt[id(sm_)] = 0
                        ksem[k] = sm_
                    maxk = max(maxk, len(ksem))
                sm_ = ksem[k]
                kcnt[id(sm_)] += 16
                sig[i] = (sm_, kcnt[id(sm_)], 16)
            elif i in need:
                cnt[o["eng"]] += 1
                sig[i] = (esem[o["eng"]], cnt[o["eng"]], 1)
        self.n_sems = len(allsems) + 5
        per = {e: [] for e in self.ENGS}
        for i, o in enumerate(ops):
            per[o["eng"]].append(i)
        block = stack.enter_context(nc.Block())

        def run(eng, e):
            waited = {}
            for i in per[e]:
                o = ops[i]
                ws = {}
                for d in o["deps"]:
                    if d not in sig:
                        continue
                    s, v, _ = sig[d]
                    if v > ws.get(id(s), (None, 0))[1]:
                        ws[id(s)] = (s, v)
                for sid, (s, v) in ws.items():
                    if waited.get(sid, 0) < v:
                        eng.wait_ge(s, v)
                        waited[sid] = v
                if o["fn"] is None:
                    continue
                ins = o["fn"](eng)
                if i in sig:
                    s, v, inc = sig[i]
                    ins.then_inc(s, inc)

        @block.sync
        def _(eng):
            run(eng, "sync")

        @block.scalar
        def _(eng):
            run(eng, "scalar")

        @block.vector
        def _(eng):
            run(eng, "vector")

        @block.gpsimd
        def _(eng):
            run(eng, "gpsimd")

        @block.tensor
        def _(eng):
            run(eng, "tensor")


class Arena:
    def __init__(self, nc, stack, nbytes):
        self.t = stack.enter_context(nc.sbuf_tensor("arena", [128, nbytes], U8))
        self.nbytes = nbytes
        self.off = 0
        self.peak = 0

    def alloc(self, n, dt, parts=128):
        sz = 4 if dt in (F32, I32) else 2
        nb = n * sz
        assert self.off + nb <= self.nbytes, ("SBUF arena overflow", self.off, nb)
        v = self.t[0:parts, self.off:self.off + nb].bitcast(dt)
        self.off += (nb + 63) // 64 * 64
        self.peak = max(self.peak, self.off)
        return v

    def mark(self):
        return self.off

    def release(self, m):
        self.off = m


def v3(ap, a, b):
    return ap.rearrange("p (a b) -> p a b", a=a, b=b)


def v4(ap, a, b, c):
    return ap.rearrange("p (a b c) -> p a b c", a=a, b=b, c=c)


def host_consts():
    c = {}
    c["ident"] = np.eye(128, dtype=np.float32)
    s_le_t = (np.arange(128)[:, None] <= np.arange(128)[None, :]).astype(np.float32)
    c["masks"] = np.concatenate([s_le_t, s_le_t.T], axis=1)
    ang = 2 * np.pi * np.outer(np.arange(128), np.arange(128)) / 128.0
    c["dft"] = np.concatenate([np.cos(ang), np.sin(ang), -np.sin(ang)], axis=1).astype(np.float32)
    angn = 2 * np.pi * np.outer(np.arange(128), np.arange(128)) / float(NFFT)
    c["tw"] = np.concatenate([np.cos(angn), np.sin(angn)], axis=1).astype(np.float32)
    n = np.arange(NFFT)
    pos = np.where(n < L, n, NFFT - n).astype(np.float64)
    t = pos / (L - 1)
    bands = np.linspace(1e-4, 16 - 1, 16).astype(np.float32).astype(np.float64)
    ang2 = (2 * math.pi / L) * pos[:, None] * bands[None]
    feats = np.concatenate([t[:, None], np.cos(ang2), -np.sin(ang2)], axis=-1)
    c["featsT"] = np.ascontiguousarray(feats.T).astype(np.float32)
    c["negt"] = np.ascontiguousarray((-t).reshape(128, 128)).astype(np.float32)
    max_decay = math.log(1e-2) / 0.3
    min_decay = math.log(1e-2) / 1.5
    c["deltas"] = np.abs(np.linspace(min_decay, max_decay, 1024, dtype=np.float32)).astype(np.float32)
    sel = np.zeros((36, 8), np.float32)
    for g in range(4):
        sel[g, g] = 1.0
        sel[32 + g, 4 + g] = 1.0
    c["sel"] = sel
    return c


def build_nc(upto="all", debug=(), skip=()):
    nc = bass.Bass("TRN2", target_bir_lowering=False)
    dbg = set(debug)

    in_names = []
    nc._in_names = in_names
    full = upto == "all"

    def din(name, shape, dt=F32, big=False):
        if big and not full:
            return None
        in_names.append(name)
        return nc.dram_tensor(name, list(shape), dt, kind="ExternalInput").ap()

    def dscr(name, shape, dt):
        kind = "ExternalOutput" if name in dbg else "Internal"
        return nc.dram_tensor(name, list(shape), dt, kind=kind).ap()

    xb = din("xb", [L, D])
    ctxb = din("ctxb", [CTX, D])
    xq = din("xq", [NQ, D])
    idxq = din("idxq", [128, 16], I32)
    cT_d = din("cT", [128, 16])
    w_mod = din("w_mod", [D, 6 * D])
    b_mod = din("b_mod", [1, 6 * D])
    g1_d = din("g1", [D])
    g2_d = din("g2", [D])
    w_in = din("w_in", [D, IN_COLS])
    b_in = din("b_in", [IN_COLS])
    qkpar_d = din("qkpar", [128, 16 * 5])
    hypar_d = din("hypar", [128, 24 * 5])
    gpar_d = din("gpar", [36, 2])
    hf_w1 = din("hf_w1", [33, 64])
    hfpar_d = din("hfpar", [128, 4])
    hf_w2 = din("hf_w2", [64, 64])
    hf_w3 = din("hf_w3", [64, 2048])
    h_bias = din("h_bias", [D])
    w_a = din("w_a", big=True, shape=[D, D])
    w_b = din("w_b", big=True, shape=[D, D])
    w_out = din("w_out", big=True, shape=[D, D])
    w_rt = din("w_rt", [D, 72])
    b_rt = din("b_rt", [72])
    w1_e = din("w1_e", big=True, shape=[64, D, 512])
    w3_e = din("w3_e", big=True, shape=[64, D, 512])
    w2_e = din("w2_e", big=True, shape=[64, 512, D])
    g_fin = din("g_fin", [D])
    c_ident = din("ident", [128, 128])
    c_masks = din("masks", [128, 256])
    c_dft = din("dft", [128, 384])
    c_tw = din("tw", [128, 256])
    c_featsT = din("featsT", [33, NFFT])
    c_negt = din("negt", [128, 128])
    c_deltas = din("deltas", [D])
    c_sel = din("sel", [36, 8])
    out = nc.dram_tensor("out", [NQ, D], F32, kind="ExternalOutput").ap()

    hT_d = dscr("hT_d", [128, 8, T], BF16)
    QK_d = dscr("QK_d", [16, 128, T], BF16)
    V_d = dscr("V_d", [T, D], BF16)
    HC_d = dscr("HC_d", [L, 3 * D], BF16)
    HF_d = HC_d[:, 0:D]
    HB_d = HC_d[:, D:2 * D]
    S_d = dscr("S_d", [L, D], BF16)
    X0_d = dscr("X0_d", [L, D], BF16)
    HY_d = HC_d[:, 2 * D:3 * D]
    G_d = dscr("G_d", [2, 2, 128, 128, 128], BF16)
    KH_d = dscr("KH_d", [2, 128, 128, 128], BF16)
    H_d = dscr("H_d", [2, 128, 128, 128], BF16)
    X1_d = dscr("X1_d", [NQ, D], F32)
    IG_d = dscr("IG_d", [36, T], F32)
    OG_d = dscr("OG_d", [NQ, 3 * D], BF16)
    H2T_d = dscr("H2T_d", [128, 8, NQ], BF16)
    WGT_d = dscr("WGT_d", [128, 16 * 64], F32)
    MOE_d = dscr("MOE_d", [NQ, D], F32)
    FG_d = dscr("FG_d", [36, T], F32)
    KERN_d = dscr("KERN_d", [128, 128, 128], BF16)
    GATE_d = dscr("GATE_d", [128, 4 * NCH * 8], F32)

    w_in_v = w_in.rearrange("(k p) n -> p k n", p=128)

    with contextlib.ExitStack() as st:
        S = Sched(nc)
        A = Arena(nc, st, 203 * 1024)
        ps = [st.enter_context(nc.psum_tensor("ps%d" % i, [128, 512], F32)) for i in range(7)]
        pb = st.enter_context(nc.psum_tensor("pb", [128, 1024], BF16))
        PS = ["ps%d" % i for i in range(7)]

        def MM(o, lhsT, rhs, start, stop, r, w):
            S.op("tensor", lambda e: e.matmul(o, lhsT=lhsT, rhs=rhs, start=start, stop=stop), r, w)

        def TR(o, i, ident, r, w):
            S.op("tensor", lambda e: e.transpose(o, i, ident), r, w)

        def ACT(o, i, func, r, w, bias=None, scale=None, accum=None):
            kw = {}
            if bias is not None:
                kw["bias"] = bias
            if scale is not None:
                kw["scale"] = scale
            if accum is not None:
                kw["accum_out"] = accum
            S.op("scalar", lambda e: e.activation(out=o, in_=i, func=func, **kw), r, w)

        def TT(eng, o, a, b, op, r, w):
            S.op(eng, lambda e: e.tensor_tensor(out=o, in0=a, in1=b, op=op), r, w)

        def TS(eng, o, a, s1, op0, r, w, s2=None, op1=None):
            if op1 is None:
                S.op(eng, lambda e: e.tensor_scalar(out=o, in0=a, scalar1=s1, scalar2=None, op0=op0), r, w)
            else:
                S.op(eng, lambda e: e.tensor_scalar(out=o, in0=a, scalar1=s1, scalar2=s2, op0=op0, op1=op1), r, w)

        def STT(eng, o, a, s, b, op0, op1, r, w):
            S.op(eng, lambda e: e.scalar_tensor_tensor(out=o, in0=a, scalar=s, in1=b, op0=op0, op1=op1), r, w)

        def CP(eng, o, i, r, w):
            if eng == "scalar":
                S.op(eng, lambda e: e.copy(out=o, in_=i), r, w)
            else:
                S.op(eng, lambda e: e.tensor_copy(out=o, in_=i), r, w)

        def MS(eng, o, val, w):
            S.op(eng, lambda e: e.memset(o, val), (), w)

        def DMA(eng, o, i, r, w):
            S.dma(eng, lambda e: e.dma_start(out=o, in_=i), r, w)

        def RED(eng, o, i, op, r, w, axis=AX.X):
            S.op(eng, lambda e: e.tensor_reduce(out=o, in_=i, axis=axis, op=op), r, w)

        def DBG(name, ap, shape, dt, r):
            if name in dbg:
                t_ = nc.dram_tensor(name, list(shape), dt, kind="ExternalOutput").ap()
                DMA("sync", t_, ap, r, [name])

        def RCP(o, i, r, w):
            S.op("vector", lambda e: e.reciprocal(out=o, in_=i), r, w)

        ident32 = A.alloc(128, F32)
        identb = A.alloc(128, BF16)
        maskb = A.alloc(256, BF16)
        dftb = A.alloc(384, BF16)
        tw = A.alloc(256, F32)
        ones32 = A.alloc(128, F32)
        onesb = A.alloc(128, BF16)
        epst = A.alloc(1, F32)
        stg = A.alloc(384, F32)
        DMA("sync", ident32, c_ident[:, :], [], ["ident32"])
        CP("vector", identb, ident32, ["ident32"], ["identb"])
        DMA("sync", stg[:, 0:256], c_masks[:, :], [], ["stg"])
        CP("vector", maskb, stg[:, 0:256], ["stg"], ["maskb"])
        DMA("sync", stg, c_dft[:, :], ["maskb"], ["stg"])
        CP("vector", dftb, stg, ["stg"], ["dftb"])
        DMA("sync", tw, c_tw[:, :], [], ["tw"])
        MS("vector", ones32, 1.0, ["ones32"])
        MS("vector", onesb, 1.0, ["onesb"])
        MS("vector", epst, EPS, ["epst"])
        Cm, Sm_, nSm = dftb[:, 0:128], dftb[:, 128:256], dftb[:, 256:384]
        GT2 = A.alloc(D, F32)
        mark_low = A.mark()
        GT1 = A.alloc(D, F32)
        G2 = A.alloc(D, F32)
        SH2 = A.alloc(D, F32)
        G1 = A.alloc(D, F32)
        SH1 = A.alloc(D, F32)

        def stageA():
            m = A.mark()
            G1c = A.alloc(D, F32)
            SH1c = A.alloc(D, F32)
            cT = A.alloc(16, F32)
            bmod = A.alloc(6 * D, F32, parts=1)
            modrow = [A.alloc(6 * D, F32, parts=1) for _ in range(2)]
            wm = [A.alloc(8 * 512, F32) for _ in range(2)]
            gbc = A.alloc(D, F32)
            DMA("sync", cT, cT_d[:, :], [], ["cT"])
            ACT(cT, cT, AF.Silu, ["cT"], ["cT"])
            DMA("sync", bmod, b_mod[:, :], [], ["bmod"])
            cT3 = v3(cT, 8, 2)
            w_mod_v = w_mod.rearrange("(k p) n -> p k n", p=128)
            for n in range(12):
                buf = wm[n % 2]
                bk = "wm%d" % (n % 2)
                DMA("sync" if n % 2 == 0 else "scalar", v3(buf, 8, 512), w_mod_v[:, :, n * 512:(n + 1) * 512], [], [bk])
                for j in range(2):
                    for k in range(8):
                        MM(ps[j][0:1, :], cT3[:, k, j:j + 1], buf[:, k * 512:(k + 1) * 512], k == 0, k == 7,
                           [bk, "cT"], [PS[j]])
                    TT("vector", modrow[j][0:1, n * 512:(n + 1) * 512], ps[j][0:1, :], bmod[0:1, n * 512:(n + 1) * 512],
                       ALU.add, [PS[j], "bmod"], ["modrow%d" % j])

            def bcast(dst, dkey, j, idx):
                for h in range(2):
                    MM(ps[2 + h][:, :], ones32[0:1, :], modrow[j][0:1, idx * D + h * 512: idx * D + (h + 1) * 512],
                       True, True, ["modrow%d" % j, "ones32"], [PS[2 + h]])
                    CP("vector", dst[:, h * 512:(h + 1) * 512], ps[2 + h][:, :], [PS[2 + h]], [dkey])

            bcast(SH1, "SH1", 0, 0)
            bcast(G1, "G1", 0, 1)
            bcast(GT1, "GT1", 0, 2)
            bcast(SH2, "SH2", 0, 3)
            bcast(G2, "G2", 0, 4)
            bcast(GT2, "GT2", 0, 5)
            bcast(SH1c, "SH1c", 1, 0)
            bcast(G1c, "G1c", 1, 1)
            DMA("sync", gbc, g1_d.partition_broadcast(128), [], ["gbc"])
            STT("vector", G1, G1, 1.0, gbc, ALU.add, ALU.mult, ["G1", "gbc"], ["G1"])
            STT("vector", G1c, G1c, 1.0, gbc, ALU.add, ALU.mult, ["G1c", "gbc"], ["G1c"])
            DMA("sync", gbc, g2_d.partition_broadcast(128), ["G1", "G1c"], ["gbc"])
            STT("vector", G2, G2, 1.0, gbc, ALU.add, ALU.mult, ["G2", "gbc"], ["G2"])
            for nm_, t_ in (("dG1", G1), ("dSH1", SH1), ("dGT1", GT1), ("dG2", G2), ("dSH2", SH2), ("dGT2", GT2), ("dG1c", G1c), ("dSH1c", SH1c)):
                DBG(nm_, t_, [128, D], F32, [nm_[1:]])
            A.release(m)
            A.off = m
            A.alloc(D, F32)
            A.alloc(D, F32)
            return G1c, SH1c

        nrm = {}

        def norm_setup(nb=3, with_x=True):
            nrm["nb"] = nb
            nrm["xt"] = [A.alloc(D, F32) for _ in range(nb)] if with_x else None
            nrm["junk"] = [A.alloc(D, BF16) for _ in range(nb)]
            nrm["t1"] = [A.alloc(D, F32) for _ in range(nb)]
            nrm["st"] = [A.alloc(8, F32) for _ in range(nb)]
            nrm["n"] = 0

        def norm_tile(src, Gt, Gk, SHt, SHk, hb, hbk, x_keep=None):
            i = nrm["n"] % nrm["nb"]
            nrm["n"] += 1
            xt = nrm["xt"][i] if x_keep is None else x_keep[0]
            xk = ("nxt%d" % i) if x_keep is None else x_keep[1]
            stt = nrm["st"][i]
            sk_, jk_, tk_ = "nst%d" % i, "njunk%d" % i, "nt1%d" % i
            if src is not None:
                DMA("sync", xt, src, [], [xk])
            MS("vector", stt[:, 0:1], 0.0, [sk_])
            ACT(nrm["junk"][i], xt, AF.Square, [xk, sk_], [jk_, sk_], accum=stt[:, 0:1])
            ACT(stt[:, 1:2], stt[:, 0:1], AF.Sqrt, [sk_, "epst"], [sk_], bias=epst[:, 0:1], scale=1.0 / D)
            RCP(stt[:, 2:3], stt[:, 1:2], [sk_], [sk_])
            STT("vector", nrm["t1"][i], xt, stt[:, 2:3], Gt, ALU.mult, ALU.mult, [xk, sk_, Gk], [tk_])
            TT("gpsimd", hb, nrm["t1"][i], SHt, ALU.add, [tk_, SHk], [hbk])

        def stageB(G1c, SH1c):
            m = A.mark()
            norm_setup()
            hb = [A.alloc(D, BF16) for _ in range(3)]
            hTt = [A.alloc(D, BF16) for _ in range(3)]
            for tt in range(NCH):
                i = tt % 3
                src = ctxb[tt * 128:(tt + 1) * 128, :] if tt < 2 else xb[(tt - 2) * 128:(tt - 1) * 128, :]
                if tt < 2:
                    norm_tile(src, G1c, "G1c", SH1c, "SH1c", hb[i], "hb%d" % i)
                else:
                    norm_tile(src, G1, "G1", SH1, "SH1", hb[i], "hb%d" % i)
                for k in range(8):
                    TR(pb[:, k * 128:(k + 1) * 128], hb[i][:, k * 128:(k + 1) * 128], identb, ["hb%d" % i, "identb"], ["pb"])
                CP("scalar", hTt[i], pb[:, :], ["pb"], ["hTt%d" % i])
                DMA("scalar", hT_d[:, :, tt * 128:(tt + 1) * 128], v3(hTt[i], 8, 128), ["hTt%d" % i], ["hT_d%d" % (tt % 4)])
            A.release(m)
            S.barrier()

        def load_w_bf16(dst3, dkey, cols, n, stage, skey, eng="sync"):
            DMA(eng, v3(stage[:, 0:8 * n], 8, n), w_in_v[:, :, cols:cols + n], [], [skey])
            CP("gpsimd", dst3, v3(stage[:, 0:8 * n], 8, n), [skey], [dkey])

        def stageC1():
            m = A.mark()
            gst = [A.alloc(512, F32) for _ in range(2)]
            stage = [A.alloc(8 * 512, F32) for _ in range(2)]
            wqk = A.alloc(8 * 2048, BF16)
            wv = A.alloc(8 * 1024, BF16)
            wg = A.alloc(8 * 72, BF16)
            wg32 = A.alloc(8 * 72, F32)
            qkpar = A.alloc(80, F32)
            gpar = A.alloc(2, F32)
            bv = A.alloc(1024, F32)
            DMA("sync", qkpar, qkpar_d[:, :], [], ["qkpar"])
            DMA("sync", gpar[0:36, :], gpar_d[:, :], [], ["gpar"])
            DMA("sync", bv, b_in[V0:V0 + 1024].partition_broadcast(128), [], ["bv"])
            wqk3 = v3(wqk, 8, 2048)
            for g in range(4):
                load_w_bf16(wqk3[:, :, g * 512:(g + 1) * 512], "wqk", g * 512, 512, stage[g % 2], "stage%d" % (g % 2),
                            "sync" if g % 2 == 0 else "scalar")
            wv3 = v3(wv, 8, 1024)
            for g in range(2):
                load_w_bf16(wv3[:, :, g * 512:(g + 1) * 512], "wv", V0 + g * 512, 512, stage[g % 2], "stage%d" % (g % 2),
                            "sync" if g % 2 == 0 else "scalar")
            MS("vector", wg32, 0.0, ["wg32"])
            wg323 = v3(wg32, 8, 72)
            with nc.allow_non_contiguous_dma(reason="tiny gate weight columns"):
                DMA("sync", wg323[:, :, 0:4], w_in_v[:, :, IG0:IG0 + 4], ["wg32"], ["wg32"])
                DMA("sync", wg323[:, :, 32:36], w_in_v[:, :, IG0 + 4:IG0 + 8], ["wg32"], ["wg32"])
                DMA("sync", wg323[:, :, 36:40], w_in_v[:, :, FG0:FG0 + 4], ["wg32"], ["wg32"])
                DMA("sync", wg323[:, :, 68:72], w_in_v[:, :, FG0 + 4:FG0 + 8], ["wg32"], ["wg32"])
            CP("vector", wg, wg32, ["wg32"], ["wg"])
            wg3 = v3(wg, 8, 72)
            hTb = [A.alloc(8 * 512, BF16) for _ in range(2)]
            zf = [A.alloc(512, F32) for _ in range(4)]
            yf = [A.alloc(512, F32) for _ in range(4)]
            qo = [A.alloc(512, BF16) for _ in range(3)]
            vo = [A.alloc(512, BF16) for _ in range(2)]
            qkbank = (0, 1, 5, 6)
            par3 = v3(qkpar, 16, 5)
            nq = 0
            nv = 0
            def c1_load(tb):
                t0_ = 0 if tb == 0 else CTX + (tb - 1) * 512
                n_ = CTX if tb == 0 else 512
                DMA("sync", v3(hTb[tb % 2], 8, 512)[:, :, 0:n_], hT_d[:, :, t0_:t0_ + n_], ["hT_d0", "hT_d1", "hT_d2", "hT_d3"],
                    ["hTb%d" % (tb % 2)])

            c1_load(0)
            for tb in range(17):
                t0 = 0 if tb == 0 else CTX + (tb - 1) * 512
                n = CTX if tb == 0 else 512
                hb_ = hTb[tb % 2]
                hk = "hTb%d" % (tb % 2)
                h3 = v3(hb_, 8, 512)
                if tb + 1 < 17:
                    c1_load(tb + 1)
                rows, rl = (1, CTX) if tb == 0 else (8, 64)
                for ct in range(16):
                    bi = ct % 4
                    pi = qkbank[bi]
                    for k in range(8):
                        MM(ps[pi][:, 0:n], wqk3[:, k, ct * 128:(ct + 1) * 128], h3[:, k, 0:n], k == 0, k == 7,
                           ["wqk", hk], [PS[pi]])
                    z = zf[bi]
                    y = yf[bi]
                    zk, yk = "zf%d" % bi, "yf%d" % bi
                    ACT(z[:, 0:n], ps[pi][:, 0:n], AF.Identity, [PS[pi], "qkpar"], [zk], bias=par3[:, ct, 4:5])
                    ACT(y[:, 0:n], z[:, 0:n], AF.Identity, [zk, "qkpar"], [yk], bias=par3[:, ct, 3:4], scale=par3[:, ct, 1:2])
                    z3 = v3(z[:, 0:n], rows, rl)
                    y3 = v3(y[:, 0:n], rows, rl)
                    STT("vector", y3[:, :, 1:rl], z3[:, :, 0:rl - 1], par3[:, ct, 0:1], y3[:, :, 1:rl], ALU.mult, ALU.add,
                        [zk, yk, "qkpar"], [yk])
                    STT("vector", y3[:, :, 0:rl - 1], z3[:, :, 1:rl], par3[:, ct, 2:3], y3[:, :, 0:rl - 1], ALU.mult, ALU.add,
                        [zk, yk, "qkpar"], [yk])
                    q = qo[nq % 3]
                    qk_ = "qo%d" % (nq % 3)
                    nq += 1
                    ACT(q[:, 0:n], y[:, 0:n], AF.Silu, [yk], [qk_])
                    DMA("scalar", QK_d[ct, :, t0:t0 + n], q[:, 0:n], [qk_], ["QK_d%d" % (ct % 4)])
                for gi, GTd in enumerate((IG_d, FG_d)):
                    for k in range(8):
                        MM(ps[2][0:36, 0:n], wg3[:, k, gi * 36:(gi + 1) * 36], h3[:, k, 0:n], k == 0, k == 7,
                           ["wg", hk], [PS[2]])
                    ACT(gst[gi][0:36, 0:n], ps[2][0:36, 0:n], AF.Identity, [PS[2], "gpar"], ["gst%d" % gi], bias=gpar[0:36, gi:gi + 1])
                    DMA("scalar", GTd[:, t0:t0 + n], gst[gi][0:36, 0:n], ["gst%d" % gi], ["G%d_d" % gi])
                for tt in range(n // 128):
                    for g in range(2):
                        pi = 3 + g
                        for k in range(8):
                            MM(ps[pi][:, :], h3[:, k, tt * 128:(tt + 1) * 128], wv3[:, k, g * 512:(g + 1) * 512], k == 0, k == 7,
                               ["wv", hk], [PS[pi]])
                        o_ = vo[nv % 2]
                        ok = "vo%d" % (nv % 2)
                        nv += 1
                        TT("vector", o_, ps[pi][:, :], bv[:, g * 512:(g + 1) * 512], ALU.add, [PS[pi], "bv"], [ok])
                        DMA("scalar", V_d[t0 + tt * 128:t0 + (tt + 1) * 128, g * 512:(g + 1) * 512], o_, [ok], ["V_d%d" % (nv % 4)])
            A.release(m)
            S.barrier()

        def stageC2():
            m = A.mark()
            stage = [A.alloc(8 * 512, F32) for _ in range(2)]
            wh = A.alloc(8 * 3072, BF16)
            wh3 = v3(wh, 8, 3072)
            hypar = A.alloc(120, F32)
            DMA("sync", hypar, hypar_d[:, :], [], ["hypar"])
            par3 = v3(hypar, 24, 5)
            for g in range(6):
                load_w_bf16(wh3[:, :, g * 512:(g + 1) * 512], "wh", HY0 + g * 512, 512, stage[g % 2], "stage%d" % (g % 2),
                            "sync" if g % 2 == 0 else "scalar")
            hTb = [A.alloc(8 * 512, BF16) for _ in range(2)]
            zf = [A.alloc(512, F32) for _ in range(6)]
            yf = [A.alloc(512, F32) for _ in range(6)]
            sb_ = [A.alloc(512, BF16) for _ in range(2)]
            x0b = [A.alloc(512, BF16) for _ in range(2)]
            tok = [A.alloc(1024, BF16) for _ in range(2)]
            cnt = 0
            def c2_load(tb):
                DMA("sync", v3(hTb[tb % 2], 8, 512), hT_d[:, :, CTX + tb * 512:CTX + (tb + 1) * 512], [], ["hTb%d" % (tb % 2)])

            c2_load(0)
            for tb in range(16):
                t0 = CTX + tb * 512
                hb_ = hTb[tb % 2]
                hk = "hTb%d" % (tb % 2)
                h3 = v3(hb_, 8, 512)
                if tb + 1 < 16:
                    c2_load(tb + 1)
                for j in range(8):
                    i2 = cnt % 2
                    cnt += 1
                    for part in range(3):
                        ct = part * 8 + j
                        pi = part + 3 * i2
                        bz = part + 3 * i2
                        for k in range(8):
                            MM(ps[pi][:, :], wh3[:, k, ct * 128:(ct + 1) * 128], h3[:, k, :], k == 0, k == 7, ["wh", hk], [PS[pi]])
                        z, y = zf[bz], yf[bz]
                        zk, yk = "hzf%d" % bz, "hyf%d" % bz
                        ACT(z, ps[pi][:, :], AF.Identity, [PS[pi], "hypar"], [zk], bias=par3[:, ct, 4:5])
                        ACT(y, z, AF.Identity, [zk, "hypar"], [yk], bias=par3[:, ct, 3:4], scale=par3[:, ct, 1:2])
                        z3, y3 = v3(z, 8, 64), v3(y, 8, 64)
                        STT("vector", y3[:, :, 1:64], z3[:, :, 0:63], par3[:, ct, 0:1], y3[:, :, 1:64], ALU.mult, ALU.add,
                            [zk, yk, "hypar"], [yk])
                        STT("vector", y3[:, :, 0:63], z3[:, :, 1:64], par3[:, ct, 2:3], y3[:, :, 0:63], ALU.mult, ALU.add,
                            [zk, yk, "hypar"], [yk])
                    CP("scalar", x0b[i2], yf[3 * i2], ["hyf%d" % (3 * i2)], ["x0b%d" % i2])
                    TT("gpsimd", sb_[i2], yf[3 * i2 + 1], yf[3 * i2 + 2], ALU.mult, ["hyf%d" % (3 * i2 + 1), "hyf%d" % (3 * i2 + 2)], ["sb%d" % i2])
                    for q in range(4):
                        TR(pb[:, q * 128:(q + 1) * 128], sb_[i2][:, q * 128:(q + 1) * 128], identb, ["sb%d" % i2, "identb"], ["pb"])
                        TR(pb[:, 512 + q * 128:512 + (q + 1) * 128], x0b[i2][:, q * 128:(q + 1) * 128], identb,
                           ["x0b%d" % i2, "identb"], ["pb"])
                    CP("scalar", tok[i2], pb[:, :], ["pb"], ["tok%d" % i2])
                    tk3 = v3(tok[i2], 8, 128)
                    DMA("scalar", S_d[tb * 512:tb * 512 + 512, j * 128:(j + 1) * 128].rearrange("(q p) c -> p q c", p=128), tk3[:, 0:4, :],
                        ["tok%d" % i2], ["S_d%d" % (cnt % 4)])
                    DMA("scalar", X0_d[tb * 512:tb * 512 + 512, j * 128:(j + 1) * 128].rearrange("(q p) c -> p q c", p=128), tk3[:, 4:8, :],
                        ["tok%d" % i2], ["X0_d%d" % (cnt % 4)])
            A.release(m)
            S.barrier()

        def gates_pre(Wt, EMTt, DECb):
            m = A.mark()
            IGT = A.alloc(T, F32)
            FGT = A.alloc(T, F32)
            NB = A.alloc(T, F32)
            G = NB
            base = A.alloc(NCH, F32)
            DMA("sync", IGT[0:36, :], IG_d[:, :], [], ["IGT"])
            DMA("scalar", FGT[0:36, :], FG_d[:, :], [], ["FGT"])
            totn = A.alloc(NCH, F32)
            mc = A.alloc(2, F32)
            dec = A.alloc(NCH, F32)
            R = A.alloc(NCH * 8, F32)
            selm = A.alloc(8, F32)
            selx = A.alloc(NCH * 8, F32)
            DMA("sync", selm[0:36, :], c_sel[:, :], [], ["selm"])
            ACT(FGT[0:36, :], FGT[0:36, :], AF.Exp, ["FGT"], ["FGT"], scale=-1.0)
            ACT(FGT[0:36, :], FGT[0:36, :], AF.Ln, ["FGT"], ["FGT"], bias=1.0)
            S.op("vector", lambda e: e.tensor_tensor_scan(out=G[0:36, :], data0=ones32[0:36, 0:1].to_broadcast([36, T]), data1=FGT[0:36, :],
                                                          initial=0.0, op0=ALU.mult, op1=ALU.add), ["ones32", "FGT"], ["NB"])
            N3 = v3(NB[0:36, :], NCH, 128)
            CP("vector", base[0:36, 0:NCH - 1], N3[:, 0:NCH - 1, 127], ["NB"], ["base"])
            TT("vector", N3[:, 1:NCH, :], N3[:, 1:NCH, :], base[0:36, 0:NCH - 1].unsqueeze(2).to_broadcast([36, NCH - 1, 128]), ALU.subtract,
               ["NB", "base"], ["NB"])
            CP("vector", totn[0:36, :], N3[:, :, 127], ["NB"], ["totn"])
            Nb = v3(NB[32:36, :], NCH, 128)
            TT("vector", Nb, totn[32:36, :].unsqueeze(2).to_broadcast([4, NCH, 128]), Nb, ALU.subtract, ["NB", "totn"], ["NB"])
            TT("vector", NB[32:36, :], NB[32:36, :], FGT[32:36, :], ALU.add, ["NB", "FGT"], ["NB"])
            TT("vector", IGT[0:36, :], IGT[0:36, :], NB[0:36, :], ALU.add, ["IGT", "NB"], ["IGT"])
            RED("vector", mc[0:36, 0:1], IGT[0:36, :], ALU.max, ["IGT"], ["mc"])
            TS("vector", mc[0:36, 1:2], mc[0:36, 0:1], -1.0, ALU.mult, ["mc"], ["mc"])
            ACT(IGT[0:36, :], IGT[0:36, :], AF.Exp, ["IGT", "mc"], ["IGT"], bias=mc[0:36, 1:2])
            TS("vector", mc[0:36, 0:1], mc[0:36, 1:2], math.log(16.0), ALU.add, ["mc"], ["mc"])
            ACT(NB[0:36, :], NB[0:36, :], AF.Exp, ["NB", "mc"], ["NB"], bias=mc[0:36, 0:1])
            ACT(dec[0:36, :], totn[0:36, :], AF.Exp, ["totn"], ["dec"], scale=-1.0)
            for src, sk, dst, dk in ((IGT, "IGT", Wt, "Wt"), (NB, "NB", EMTt, "EMTt")):
                for half in range(2):
                    for cc in range(33):
                        c = half * 33 + cc
                        MM(ps[half][:, cc * 8:(cc + 1) * 8], src[0:36, c * 128:(c + 1) * 128], selm[0:36, :], True, True,
                           [sk, "selm"], [PS[half]])
                    CP("vector", dst[:, half * 264:(half + 1) * 264], ps[half][:, 0:264], [PS[half]], [dk])
            MS("vector", R[0:36, :], 0.0, ["R"])
            CP("vector", v3(selx[0:36, :], NCH, 8), selm[0:36, :].unsqueeze(1).to_broadcast([36, NCH, 8]), ["selm"], ["selx"])
            TT("vector", v3(R[0:36, :], NCH, 8), v3(selx[0:36, :], NCH, 8), dec[0:36, :].unsqueeze(2).to_broadcast([36, NCH, 8]),
               ALU.mult, ["selx", "dec"], ["R"])
            for half in range(2):
                MM(ps[2 + half][:, 0:264], ones32[0:36, :], R[0:36, half * 264:(half + 1) * 264], True, True, ["R", "ones32"],
                   [PS[2 + half]])
                CP("vector", DECb[:, half * 264:(half + 1) * 264], ps[2 + half][:, 0:264], [PS[2 + half]], ["DECb"])
            if "GATE_d" in dbg:
                DMA("sync", GATE_d[:, 0:528], Wt, ["Wt"], ["GATE_d"])
                DMA("sync", GATE_d[:, 528:1056], EMTt, ["EMTt"], ["GATE_d"])
                DMA("sync", GATE_d[:, 1056:1584], DECb, ["DECb"], ["GATE_d"])
            A.release(m)
            S.barrier()

        def stageD():
            m0 = A.mark()
            Wt = A.alloc(NCH * 8, F32)
            EMTt = A.alloc(NCH * 8, F32)
            DECb = A.alloc(NCH * 8, F32)
            gates_pre(Wt, EMTt, DECb)
            QT = A.alloc(2 * T, BF16)
            KT = A.alloc(2 * T, BF16)
            Vx = A.alloc(NCH * 258, BF16)
            Ktok = A.alloc(NCH * 256, BF16)
            Est = [A.alloc(514, F32) for _ in range(2)]
            Cb = [[A.alloc(514, BF16) for _ in range(2)] for _ in range(2)]
            Vp = [A.alloc(257, BF16) for _ in range(4)]
            Smk = [A.alloc(128, BF16) for _ in range(4)]
            ho = [A.alloc(256, BF16) for _ in range(4)]
            sm = [A.alloc(4, F32) for _ in range(4)]
            QT3, KT3 = v3(QT, 2, T), v3(KT, 2, T)
            Vx3 = v3(Vx, NCH, 258)
            Kt3 = v3(Ktok, NCH, 256)
            order = [list(range(NCH)), [1, 0] + list(range(NCH - 1, 1, -1))]
            step = 0
            for h in range(4):
                DMA("sync", QT3, QK_d[2 * h:2 * h + 2, :, :].rearrange("c p t -> p c t"), [], ["QT"])
                DMA("scalar", KT3, QK_d[8 + 2 * h:8 + 2 * h + 2, :, :].rearrange("c p t -> p c t"), [], ["KT"])
                DMA("sync", Vx3[:, :, 0:256], V_d[:, h * 256:(h + 1) * 256].rearrange("(c p) e -> p c e", p=128), [], ["Vx"])
                MS("vector", Vx3[:, :, 256:257], 1.0, ["Vx1"])
                for c0 in range(0, NCH, 4):
                    nn = min(4, NCH - c0)
                    for cc in range(nn):
                        for dh in range(2):
                            TR(pb[:, (cc * 2 + dh) * 128:(cc * 2 + dh + 1) * 128], KT3[:, dh, (c0 + cc) * 128:(c0 + cc + 1) * 128], identb,
                               ["KT", "identb"], ["pb"])
                    CP("scalar", Ktok[:, c0 * 256:(c0 + nn) * 256], pb[:, 0:nn * 256], ["pb"], ["Ktok"])
                for d in range(2):
                    MS("vector", Est[d], 0.0, ["Est%d" % d])
                    MS("gpsimd", Cb[d][0], 0.0, ["Cb%d0" % d])
                prev = [None, None]
                steps = [(i, d) for i in range(NCH) for d in range(2)]
                step0 = step

                def stepA(n):
                    i, d = steps[n]
                    c = order[d][i]
                    col = c * 8 + d * 4 + h
                    r4 = (step0 + n) % 4
                    vp, vpk = Vp[r4], "Vp%d" % r4
                    TS("vector", vp, Vx3[:, c, 0:257], Wt[:, col:col + 1], ALU.mult, ["Vx", "Vx1", "Wt"], [vpk])
                    if c >= 2:
                        for dh in range(2):
                            MM(ps[0][:, 0:128], KT3[:, dh, c * 128:(c + 1) * 128], QT3[:, dh, c * 128:(c + 1) * 128], dh == 0, dh == 1,
                               ["KT", "QT"], [PS[0]])
                        TT("vector", Smk[r4], ps[0][:, 0:128], maskb[:, d * 128:(d + 1) * 128], ALU.mult, [PS[0], "maskb"], ["Smk%d" % r4])

                def stepB(n):
                    i, d = steps[n]
                    c = order[d][i]
                    g = d * 4 + h
                    col = c * 8 + g
                    r4 = (step0 + n) % 4
                    cbk = "Cb%d%d" % (d, i % 2)
                    cbn = "Cb%d%d" % (d, (i + 1) % 2)
                    vp, vpk = Vp[r4], "Vp%d" % r4
                    lat = c >= 2
                    if lat:
                        smk, smkk = Smk[r4], "Smk%d" % r4
                        pn = ps[1 + d]
                        MM(pn[:, 0:257], smk, vp, True, False, [smkk, vpk], [PS[1 + d]])
                        for dh in range(2):
                            MM(pn[:, 0:257], QT3[:, dh, c * 128:(c + 1) * 128], Cb[d][i % 2][:, dh * 257:(dh + 1) * 257], False, dh == 1,
                               ["QT", cbk], [PS[1 + d]])
                    for dh in range(2):
                        pu = ps[3 + d * 2 + dh]
                        MM(pu[:, 0:257], Kt3[:, c, dh * 128:(dh + 1) * 128], vp, True, True, ["Ktok", vpk], [PS[3 + d * 2 + dh]])
                        pc = prev[d] if prev[d] is not None else col
                        STT("vector", Est[d][:, dh * 257:(dh + 1) * 257], Est[d][:, dh * 257:(dh + 1) * 257], DECb[:, pc:pc + 1],
                            pu[:, 0:257], ALU.mult, ALU.add, ["Est%d" % d, "DECb", PS[3 + d * 2 + dh]], ["Est%d" % d])
                    prev[d] = col
                    ACT(Cb[d][(i + 1) % 2], Est[d], AF.Copy, ["Est%d" % d, "DECb"], [cbn], scale=DECb[:, col:col + 1])
                    if lat:
                        s_ = sm[r4]
                        sk = "sm%d" % r4
                        TS("vector", s_[:, 3:4], pn[:, 256:257], -1.0, ALU.mult, [PS[1 + d]], [sk])
                        TT("vector", s_[:, 0:1], pn[:, 256:257], s_[:, 3:4], ALU.max, [PS[1 + d], sk], [sk])
                        TT("vector", s_[:, 1:2], s_[:, 0:1], EMTt[:, col:col + 1], ALU.max, [sk, "EMTt"], [sk])
                        RCP(s_[:, 2:3], s_[:, 1:2], [sk], [sk])
                        ACT(ho[r4], pn[:, 0:256], AF.Copy, [PS[1 + d], sk], ["ho%d" % r4], scale=s_[:, 2:3])
                        dst = (HF_d, HB_d)[d]
                        DMA("sync" if d == 0 else "scalar", dst[(c - 2) * 128:(c - 1) * 128, h * 256:(h + 1) * 256], ho[r4],
                            ["ho%d" % r4], ["H%d_d%d" % (d, i % 4)])

                stepA(0)
                for n in range(len(steps)):
                    if n + 1 < len(steps):
                        stepA(n + 1)
                    stepB(n)
                step += len(steps)
            A.release(m0)
            S.barrier()


        def cmul_store(pa, pa_k, pb_, pb_k, cr, ci, ck, sign, dst_re, dst_im, dre_k, dim_k, tmp, tk):
            t1, t2, t3, t4 = tmp
            TT("vector", t1, pa, cr, ALU.mult, [pa_k] + ck, [tk + "1"])
            TT("vector", t2, pb_, ci, ALU.mult, [pb_k] + ck, [tk + "2"])
            TT("vector", t3, pa, ci, ALU.mult, [pa_k] + ck, [tk + "3"])
            TT("vector", t4, pb_, cr, ALU.mult, [pb_k] + ck, [tk + "4"])
            if sign > 0:
                TT("gpsimd", dst_re, t1, t2, ALU.subtract, [tk + "1", tk + "2"], [dre_k])
                TT("vector", dst_im, t3, t4, ALU.add, [tk + "3", tk + "4"], [dim_k])
            else:
                TT("gpsimd", dst_re, t1, t2, ALU.add, [tk + "1", tk + "2"], [dre_k])
                TT("gpsimd", dst_im, t4, t3, ALU.subtract, [tk + "3", tk + "4"], [dim_k])

        def stageE():
            m0 = A.mark()
            PI = math.pi
            hid = A.alloc(NFFT, BF16)
            negt = A.alloc(128, F32)
            hfpar = A.alloc(4, F32)
            fb = A.alloc(2, F32)
            negpi = A.alloc(1, F32)
            w1t = A.alloc(64, F32)
            w2d = A.alloc(128, F32)
            DMA("sync", negt, c_negt[:, :], [], ["negt"])
            DMA("sync", hfpar, hfpar_d[:, :], [], ["hfpar"])
            DMA("sync", w1t[0:33, :], hf_w1[:, :], [], ["w1t"])
            DMA("sync", w2d[0:64, 0:64], hf_w2[:, :], [], ["w2d"])
            DMA("sync", w2d[0:64, 64:128], hf_w2[:, :], [], ["w2d"])
            MS("vector", negpi, -PI, ["negpi"])
            TT("vector", fb[:, 0:1], hfpar[:, 0:1], hfpar[:, 2:3], ALU.mult, ["hfpar"], ["fb"])
            TT("vector", fb[:, 1:2], hfpar[:, 1:2], hfpar[:, 2:3], ALU.mult, ["hfpar"], ["fb"])
            mm_ = A.mark()
            fch = [A.alloc(512, F32) for _ in range(2)]
            arg = [A.alloc(512, F32) for _ in range(2)]
            h1 = [A.alloc(512, F32) for _ in range(2)]
            ni = A.alloc(512, I32)
            nf = A.alloc(512, F32)
            mk = A.alloc(512, F32)

            def rr(x, xk, P_):
                TS("vector", x, x, 1.0 / (2 * PI), ALU.mult, [xk], [xk], s2=16.5, op1=ALU.add)
                CP("vector", ni[0:P_, :], x, [xk], ["rr_ni"])
                CP("vector", nf[0:P_, :], ni[0:P_, :], ["rr_ni"], ["rr_nf"])
                TT("vector", x, x, nf[0:P_, :], ALU.subtract, [xk, "rr_nf"], [xk])
                TS("vector", mk[0:P_, :], x, -1e12, ALU.mult, [xk], ["rr_mk"])
                TS("vector", mk[0:P_, :], mk[0:P_, :], 0.0, ALU.max, ["rr_mk"], ["rr_mk"], s2=1.0, op1=ALU.min)
                TT("vector", x, x, mk[0:P_, :], ALU.add, [xk, "rr_mk"], [xk])

            for q in range(32):
                i = q % 2
                DMA("sync", fch[i][0:33, :], c_featsT[:, q * 512:(q + 1) * 512], [], ["fch%d" % i])
                MM(ps[0][0:64, :], w1t[0:33, 0:64], fch[i][0:33, :], True, True, ["w1t", "fch%d" % i], [PS[0]])
                TS("vector", arg[i][0:64, :], ps[0][0:64, :], hfpar[0:64, 2:3], ALU.mult, [PS[0], "hfpar", "fb"], ["arg%d" % i],
                   s2=fb[0:64, 0:1], op1=ALU.add)
                rr(arg[i][0:64, :], "arg%d" % i, 64)
                ACT(h1[i][0:64, :], arg[i][0:64, :], AF.Sin, ["arg%d" % i, "negpi"], ["h1%d" % i], bias=negpi[0:64, 0:1], scale=2 * PI)
                MM(ps[1][:, :], w2d[0:64, :], h1[i][0:64, :], True, True, ["w2d", "h1%d" % i], [PS[1]])
                TS("vector", arg[i], ps[1][:, :], hfpar[:, 2:3], ALU.mult, [PS[1], "hfpar", "fb"], ["arg%d" % i], s2=fb[:, 1:2], op1=ALU.add)
                rr(arg[i], "arg%d" % i, 128)
                ACT(hid[:, q * 512:(q + 1) * 512], arg[i], AF.Sin, ["arg%d" % i, "negpi"], ["hid"], bias=negpi[:, 0:1], scale=2 * PI)
            MS("vector", hid[0:64, L:NFFT], 0.0, ["hid"])
            MS("vector", hid[64:128, 0:L + 1], 0.0, ["hid"])
            A.release(mm_)
            kbx = A.alloc(NFFT, BF16)
            xs = A.alloc(NFFT, BF16)
            w3s = A.alloc(128, F32)
            w3a = A.alloc(128, BF16)
            dbc = A.alloc(128, F32)
            hbc = A.alloc(128, F32)
            rnb = A.alloc(128, F32)
            ssr = A.alloc(512, F32, parts=1)
            win = [A.alloc(512, F32) for _ in range(2)]
            sq = [A.alloc(512, BF16) for _ in range(2)]
            tmpP = [[A.alloc(512, F32) for _ in range(4)] for _ in range(2)]
            tmp2P = [[A.alloc(512, F32) for _ in range(4)] for _ in range(2)]
            zbP = [[A.alloc(512, BF16) for _ in range(2)] for _ in range(2)]
            ob = [[A.alloc(512, BF16) for _ in range(2)] for _ in range(2)]
            ib = [[A.alloc(512, BF16) for _ in range(2)] for _ in range(2)]
            kb2 = [[A.alloc(512, BF16) for _ in range(2)] for _ in range(2)]
            hyo = [A.alloc(512, BF16) for _ in range(2)]
            tcv = lambda q: tw[:, 4 * q:4 * q + 4].unsqueeze(2).to_broadcast([128, 4, 128])
            tsv = lambda q: tw[:, 128 + 4 * q:128 + 4 * q + 4].unsqueeze(2).to_broadcast([128, 4, 128])
            p3 = lambda t_: v3(t_, 4, 128)
            Gk = lambda kind: ["G%d_w%d" % (kind, j) for j in range(4)]
            S_v = S_d.rearrange("(a b) c -> a b c", b=128)
            X0_v = X0_d.rearrange("(a b) c -> a b c", b=128)
            HY_v = HY_d.rearrange("(a b) c -> a b c", b=128)

            def fwd12(src, K, kind, skey):
                for q in range(32):
                    i = q % 2
                    a_, b_ = 2 * i, 2 * i + 1
                    MM(ps[a_][:, :], Cm[0:K, :], src[0:K, q * 512:(q + 1) * 512], True, True, ["dftb", skey], [PS[a_]])
                    MM(ps[b_][:, :], Sm_[0:K, :], src[0:K, q * 512:(q + 1) * 512], True, True, ["dftb", skey], [PS[b_]])
                    t1, t2, t3, t4 = tmpP[i]
                    tk = "tmp%d_" % i
                    TT("vector", p3(t1), p3(ps[a_][:, :]), tcv(q), ALU.mult, [PS[a_], "tw"], [tk + "1"])
                    TT("vector", p3(t2), p3(ps[b_][:, :]), tsv(q), ALU.mult, [PS[b_], "tw"], [tk + "2"])
                    TT("vector", p3(t3), p3(ps[b_][:, :]), tcv(q), ALU.mult, [PS[b_], "tw"], [tk + "3"])
                    TT("vector", p3(t4), p3(ps[a_][:, :]), tsv(q), ALU.mult, [PS[a_], "tw"], [tk + "4"])
                    TT("gpsimd", ob[i][0], t1, t2, ALU.subtract, [tk + "1", tk + "2"], ["ob%d0" % i])
                    STT("vector", ob[i][1], t3, -1.0, t4, ALU.mult, ALU.subtract, [tk + "3", tk + "4"], ["ob%d1" % i])
                    for comp in range(2):
                        DMA("scalar", G_d[kind, comp, :, 4 * q:4 * q + 4, :], p3(ob[i][comp]),
                            ["ob%d%d" % (i, comp)], ["G%d_w%d" % (kind, (2 * q + comp) % 4)])

            def fwd34_load(kind, q):
                i = q % 2
                for comp in range(2):
                    DMA("sync", p3(ib[i][comp]), G_d[kind, comp, 4 * q:4 * q + 4, :, :].rearrange("k b c -> b k c"),
                        Gk(kind), ["ib%d%d" % (i, comp)])

            def fwd34(kind, q, a_, b_):
                i = q % 2
                gre, gim = ib[i]
                MM(ps[a_][:, :], Cm, gre, True, False, ["dftb", "ib%d0" % i], [PS[a_]])
                MM(ps[a_][:, :], Sm_, gim, False, True, ["dftb", "ib%d1" % i], [PS[a_]])
                MM(ps[b_][:, :], Cm, gim, True, False, ["dftb", "ib%d1" % i], [PS[b_]])
                MM(ps[b_][:, :], nSm, gre, False, True, ["dftb", "ib%d0" % i], [PS[b_]])

            for cb in range(8):
                c0 = cb * 128
                DMA("sync", w3s[0:64, :], hf_w3[:, c0:c0 + 128], [], ["w3s"])
                DMA("sync", w3s[64:128, :], hf_w3[:, D + c0:D + c0 + 128], [], ["w3s"])
                CP("vector", w3a, w3s, ["w3s"], ["w3a"])
                DMA("sync", dbc, c_deltas[c0:c0 + 128].partition_broadcast(128), [], ["dbc"])
                DMA("sync", hbc, h_bias[c0:c0 + 128].partition_broadcast(128), [], ["hbc"])
                for q in range(32):
                    i = q % 2
                    pk = 5 - i
                    for bb in range(4):
                        b = 4 * q + bb
                        MM(ps[pk][:, bb * 128:(bb + 1) * 128], hid[:, b:NFFT:128], w3a, True, True, ["hid", "w3a"], [PS[pk]])
                        ACT(win[i][:, bb * 128:(bb + 1) * 128], dbc, AF.Exp, ["dbc", "negt"], ["win%d" % i], scale=negt[:, b:b + 1])
                    TT("vector", kbx[:, q * 512:(q + 1) * 512], ps[pk][:, :], win[i], ALU.mult, [PS[pk], "win%d" % i], ["kbx"])
                    TT("gpsimd", sq[i], kbx[:, q * 512:(q + 1) * 512], kbx[:, q * 512:(q + 1) * 512], ALU.mult, ["kbx"], ["sq%d" % i])
                    MM(ps[6][0:1, :], onesb[:, 0:1], sq[i], q == 0, q == 31, ["onesb", "sq%d" % i], [PS[6]])
                if "KERN_d" in dbg and cb == 0:
                    DMA("sync", KERN_d[:, :, :], v3(kbx, 128, 128), ["kbx"], ["KERN_d"])
                CP("vector", ssr[0:1, :], ps[6][0:1, :], [PS[6]], ["ssr"])
                TT("vector", ssr[0:1, 0:256], ssr[0:1, 0:256], ssr[0:1, 256:512], ALU.add, ["ssr"], ["ssr"])
                TT("vector", ssr[0:1, 0:128], ssr[0:1, 0:128], ssr[0:1, 128:256], ALU.add, ["ssr"], ["ssr"])
                ACT(ssr[0:1, 128:256], ssr[0:1, 0:128], AF.Sqrt, ["ssr", "epst"], ["ssr"], bias=epst[0:1, 0:1])
                RCP(ssr[0:1, 256:384], ssr[0:1, 128:256], ["ssr"], ["ssr"])
                TS("vector", ssr[0:1, 256:384], ssr[0:1, 256:384], 1.0 / NFFT, ALU.mult, ["ssr"], ["ssr"])
                MM(ps[6][0:64, 0:128], ones32[0:1, 0:64], ssr[0:1, 256:384], True, True, ["ones32", "ssr"], [PS[6]])
                CP("vector", rnb[0:64, :], ps[6][0:64, 0:128], [PS[6]], ["rnb"])
                DMA("sync", v3(xs[0:64, :], 128, 128), S_v[:, :, c0:c0 + 128], ["S_d%d" % j for j in range(4)], ["xs"])
                fwd12(kbx, 128, 1, "kbx")
                fwd12(xs, 64, 0, "xs")
                fwd34_load(1, 0)
                for q in range(32):
                    i = q % 2
                    if q + 1 < 32:
                        fwd34_load(1, q + 1)
                    fwd34(1, q, 2 * i, 2 * i + 1)
                    CP("scalar", kb2[i][0], ps[2 * i][:, :], [PS[2 * i]], ["kb2%d0" % i])
                    CP("scalar", kb2[i][1], ps[2 * i + 1][:, :], [PS[2 * i + 1]], ["kb2%d1" % i])
                    for comp in range(2):
                        DMA("scalar", KH_d[comp, :, 4 * q:4 * q + 4, :], p3(kb2[i][comp]), ["kb2%d%d" % (i, comp)],
                            ["KH_w%d" % ((2 * q + comp) % 4)])
                DMA("sync", v3(kbx[0:64, :], 128, 128), X0_v[:, :, c0:c0 + 128], ["X0_d%d" % j for j in range(4)], ["kbx"])
                def sig_load(q):
                    fwd34_load(0, q)
                    for comp in range(2):
                        DMA("sync", p3(kb2[q % 2][comp]), KH_d[comp, :, 4 * q:4 * q + 4, :],
                            ["KH_w%d" % j for j in range(4)], ["kb2%d%d" % (q % 2, comp)])

                sig_load(0)
                for q in range(32):
                    i = q % 2
                    if q + 1 < 32:
                        sig_load(q + 1)
                    xa_, xb_ = (0, 1) if i == 0 else (4, 5)
                    fwd34(0, q, xa_, xb_)
                    zb = zbP[i]
                    tmp = tmpP[i]
                    tmp2 = tmp2P[i]
                    cmul_store(ps[xa_][:, :], PS[xa_], ps[xb_][:, :], PS[xb_], kb2[i][0], kb2[i][1], ["kb2%d0" % i, "kb2%d1" % i], +1,
                               zb[0], zb[1], "zb%d0" % i, "zb%d1" % i, tmp, "tmp%d_" % i)
                    MM(ps[2][:, :], Cm, zb[0], True, False, ["dftb", "zb%d0" % i], [PS[2]])
                    MM(ps[2][:, :], nSm, zb[1], False, True, ["dftb", "zb%d1" % i], [PS[2]])
                    MM(ps[3][:, :], Cm, zb[1], True, False, ["dftb", "zb%d1" % i], [PS[3]])
                    MM(ps[3][:, :], Sm_, zb[0], False, True, ["dftb", "zb%d0" % i], [PS[3]])
                    t1, t2, t3, t4 = tmp2
                    qk_ = "tmq%d_" % i
                    TT("vector", p3(t1), p3(ps[2][:, :]), tcv(q), ALU.mult, [PS[2], "tw"], [qk_ + "1"])
                    TT("vector", p3(t2), p3(ps[3][:, :]), tsv(q), ALU.mult, [PS[3], "tw"], [qk_ + "2"])
                    TT("vector", p3(t3), p3(ps[2][:, :]), tsv(q), ALU.mult, [PS[2], "tw"], [qk_ + "3"])
                    TT("vector", p3(t4), p3(ps[3][:, :]), tcv(q), ALU.mult, [PS[3], "tw"], [qk_ + "4"])
                    TT("gpsimd", ob[i][0], t1, t2, ALU.subtract, [qk_ + "1", qk_ + "2"], ["ob%d0" % i])
                    TT("vector", ob[i][1], t3, t4, ALU.add, [qk_ + "3", qk_ + "4"], ["ob%d1" % i])
                    for comp in range(2):
                        DMA("scalar", H_d[comp, :, 4 * q:4 * q + 4, :], p3(ob[i][comp]), ["ob%d%d" % (i, comp)],
                            ["H_w%d" % ((2 * q + comp) % 4)])
                def fin_load(q):
                    for comp in range(2):
                        DMA("sync", p3(ib[q % 2][comp]), H_d[comp, 4 * q:4 * q + 4, :, :].rearrange("b k c -> k b c"),
                            ["H_w%d" % j for j in range(4)], ["ib%d%d" % (q % 2, comp)])

                fin_load(0)
                for q in range(32):
                    i = q % 2
                    if q + 1 < 32:
                        fin_load(q + 1)
                    pe_ = 4 + i
                    MM(ps[pe_][0:64, :], Cm[:, 0:64], ib[i][0], True, False, ["dftb", "ib%d0" % i], [PS[pe_]])
                    MM(ps[pe_][0:64, :], nSm[:, 0:64], ib[i][1], False, True, ["dftb", "ib%d1" % i], [PS[pe_]])
                    t1, t2 = tmpP[i][0], tmpP[i][1]
                    k1_, k2_ = "tmp%d_1" % i, "tmp%d_2" % i
                    TT("vector", p3(t1[0:64, :]), p3(ps[pe_][0:64, :]), rnb[0:64, :].unsqueeze(1).to_broadcast([64, 4, 128]), ALU.mult,
                       [PS[pe_], "rnb"], [k1_])
                    TT("gpsimd", p3(t2[0:64, :]), p3(xs[0:64, q * 512:(q + 1) * 512]), hbc[0:64, :].unsqueeze(1).to_broadcast([64, 4, 128]),
                       ALU.mult, ["xs", "hbc"], [k2_])
                    TT("gpsimd", t1[0:64, :], t1[0:64, :], t2[0:64, :], ALU.add, [k1_, k2_], [k1_])
                    TT("vector", hyo[i][0:64, :], t1[0:64, :], kbx[0:64, q * 512:(q + 1) * 512], ALU.mult, [k1_, "kbx"], ["hyo%d" % i])
                    DMA("scalar", HY_v[:, 4 * q:4 * q + 4, c0:c0 + 128], p3(hyo[i][0:64, :]), ["hyo%d" % i], ["HY_d%d" % (q % 4)])
            A.release(m0)
            S.barrier()

        def transpose8(src_b, skey, dst_b, dkey):
            for k in range(8):
                TR(pb[:, k * 128:(k + 1) * 128], src_b[:, k * 128:(k + 1) * 128], identb, [skey, "identb"], ["pb"])
            CP("scalar", dst_b, pb[:, :], ["pb"], [dkey])

        def stageF1():
            m = A.mark()
            norm_setup()
            stage = [A.alloc(8 * 512, F32) for _ in range(2)]
            wph = A.alloc(8 * 3072, BF16)
            wph3 = v3(wph, 8, 3072)
            bph = A.alloc(3072, F32)
            for part, c_ in enumerate((O0, GA0, GB0)):
                DMA("sync", bph[:, part * D:(part + 1) * D], b_in[c_:c_ + D].partition_broadcast(128), [], ["bph"])
                for g in range(2):
                    gg = part * 2 + g
                    load_w_bf16(wph3[:, :, gg * 512:(gg + 1) * 512], "wph", c_ + g * 512, 512, stage[gg % 2], "stage%d" % (gg % 2),
                                "sync" if gg % 2 == 0 else "scalar")
            hb = [A.alloc(D, BF16) for _ in range(2)]
            hT = [A.alloc(D, BF16) for _ in range(2)]
            tf = [A.alloc(512, F32) for _ in range(2)]
            og = [A.alloc(3072, BF16) for _ in range(2)]
            for tt in range(16):
                i = tt % 2
                norm_tile(xq[tt * 128:(tt + 1) * 128, :], G1, "G1", SH1, "SH1", hb[i], "fhb%d" % i)
                transpose8(hb[i], "fhb%d" % i, hT[i], "fhT%d" % i)
                h3 = v3(hT[i], 8, 128)
                for gg in range(6):
                    pi = gg % 2
                    for k in range(8):
                        MM(ps[pi][:, :], h3[:, k, :], wph3[:, k, gg * 512:(gg + 1) * 512], k == 0, k == 7, ["fhT%d" % i, "wph"], [PS[pi]])
                    TT("vector", tf[pi], ps[pi][:, :], bph[:, gg * 512:(gg + 1) * 512], ALU.add, [PS[pi], "bph"], ["tf%d" % pi])
                    ACT(og[i][:, gg * 512:(gg + 1) * 512], tf[pi], AF.Sigmoid, ["tf%d" % pi], ["og%d" % i])
                DMA("sync", OG_d[tt * 128:(tt + 1) * 128, :], og[i], ["og%d" % i], ["OG_d%d" % i])
            A.release(m)
            S.barrier()

        ngath = [0]

        def gather(dst, dkey, src2d, idxcol, nrows, rkeys):
            ngath[0] += 1
            S.dma("gpsimd", lambda e: e.indirect_dma_start(out=dst, out_offset=None, in_=src2d,
                                                           in_offset=bass.IndirectOffsetOnAxis(ap=idxcol, axis=0),
                                                           bounds_check=nrows - 1, oob_is_err=False), rkeys, [dkey, "indslot%d" % (ngath[0] % 2)])

        def stageF2():
            m = A.mark()
            norm_setup(1, with_x=False)
            stage = [A.alloc(8 * 512, F32) for _ in range(2)]
            wabo = [A.alloc(8 * D, BF16) for _ in range(3)]
            for wi, wsrc in enumerate((w_a, w_b, w_out)):
                wv_ = wsrc.rearrange("(k p) n -> p k n", p=128)
                for g in range(2):
                    st_ = stage[(wi * 2 + g) % 2]
                    sk = "stage%d" % ((wi * 2 + g) % 2)
                    DMA("sync" if g == 0 else "scalar", v3(st_, 8, 512), wv_[:, :, g * 512:(g + 1) * 512], [], [sk])
                    CP("gpsimd", v3(wabo[wi], 8, D)[:, :, g * 512:(g + 1) * 512], v3(st_, 8, 512), [sk], ["wabo%d" % wi])
            wr32 = A.alloc(8 * 72, F32)
            brt = A.alloc(72, F32)
            idx = st.enter_context(nc.sbuf_tensor("idxt", [128, 16], I32))
            DMA("sync", v3(wr32, 8, 72), w_rt.rearrange("(k p) n -> p k n", p=128), [], ["wr32"])
            DMA("sync", brt, b_rt.partition_broadcast(128), [], ["brt"])
            DMA("sync", idx[:, :], idxq[:, :], [], ["idx"])
            og = [A.alloc(3072, BF16) for _ in range(2)]
            gthc = [A.alloc(3 * D, BF16) for _ in range(2)]
            gth = [[g_[:, 0:D], g_[:, D:2 * D], g_[:, 2 * D:3 * D]] for g_ in gthc]
            ab = A.alloc(D, BF16)
            aT = A.alloc(D, BF16)
            hyT = A.alloc(D, BF16)
            m1 = A.alloc(D, F32)
            m2 = A.alloc(512, F32)
            mb = A.alloc(D, BF16)
            mT = A.alloc(D, BF16)
            xres = A.alloc(D, F32)
            x1t = A.alloc(D, F32)
            h2f = A.alloc(D, F32)
            h2b = A.alloc(D, BF16)
            h2T = A.alloc(D, BF16)
            h2T32 = A.alloc(D, F32)
            wg_all = A.alloc(16 * 64, F32)
            rt = A.alloc(256, F32)
            wa3, wb3, wo3 = (v3(w_, 8, D) for w_ in wabo)
            for tt in range(16):
                i = tt % 2
                DMA("sync", og[i], OG_d[tt * 128:(tt + 1) * 128, :], ["OG_d0", "OG_d1"], ["fog%d" % i])
                DMA("sync", xres, xq[tt * 128:(tt + 1) * 128, :], [], ["xres"])
                hk_ = ["H0_d%d" % j for j in range(4)] + ["H1_d%d" % j for j in range(4)]
                gather(gthc[i], "gth%d" % i, HC_d[:, :], idx[:, tt:tt + 1], L, ["idx"] + hk_ + ["HY_d%d" % j for j in range(4)])
                TT("gpsimd", m1, gth[i][0], gth[i][1], ALU.add, ["gth%d" % i], ["m1"])
                TT("vector", ab, m1, og[i][:, 0:D], ALU.mult, ["m1", "fog%d" % i], ["ab"])
                transpose8(ab, "ab", aT, "aT")
                transpose8(gth[i][2], "gth%d" % i, hyT, "hyT")
                a3, y3 = v3(aT, 8, 128), v3(hyT, 8, 128)
                for half in range(2):
                    for k in range(8):
                        MM(ps[0][:, :], a3[:, k, :], wa3[:, k, half * 512:(half + 1) * 512], k == 0, k == 7, ["aT", "wabo0"], [PS[0]])
                    for k in range(8):
                        MM(ps[1][:, :], y3[:, k, :], wb3[:, k, half * 512:(half + 1) * 512], k == 0, k == 7, ["hyT", "wabo1"], [PS[1]])
                    TT("vector", m1[:, half * 512:(half + 1) * 512], ps[0][:, :], og[i][:, D + half * 512:D + (half + 1) * 512], ALU.mult,
                       [PS[0], "fog%d" % i], ["m1"])
                    TT("vector", m2, ps[1][:, :], og[i][:, 2 * D + half * 512:2 * D + (half + 1) * 512], ALU.mult, [PS[1], "fog%d" % i], ["m2"])
                    TT("gpsimd", mb[:, half * 512:(half + 1) * 512], m1[:, half * 512:(half + 1) * 512], m2, ALU.add, ["m1", "m2"], ["mb"])
                transpose8(mb, "mb", mT, "mT")
                mT3 = v3(mT, 8, 128)
                for half in range(2):
                    for k in range(8):
                        MM(ps[2][:, :], mT3[:, k, :], wo3[:, k, half * 512:(half + 1) * 512], k == 0, k == 7, ["mT", "wabo2"], [PS[2]])
                    TT("vector", m1[:, half * 512:(half + 1) * 512], ps[2][:, :], GT1[:, half * 512:(half + 1) * 512], ALU.mult,
                       [PS[2], "GT1"], ["m1"])
                TT("gpsimd", x1t, m1, xres, ALU.add, ["m1", "xres"], ["x1t"])
                DMA("sync", X1_d[tt * 128:(tt + 1) * 128, :], x1t, ["x1t"], ["X1_d%d" % i])
                norm_tile(None, G2, "G2", SH2, "SH2", h2f, "h2f", x_keep=(x1t, "x1t"))
                CP("vector", h2b, h2f, ["h2f"], ["h2b"])
                transpose8(h2b, "h2b", h2T, "h2T")
                DMA("sync", H2T_d[:, :, tt * 128:(tt + 1) * 128], v3(h2T, 8, 128), ["h2T"], ["H2T_d%d" % i])
                for k in range(8):
                    TR(ps[3 + k // 4][:, (k % 4) * 128:(k % 4 + 1) * 128], h2f[:, k * 128:(k + 1) * 128], ident32, ["h2f", "ident32"],
                       [PS[3 + k // 4]])
                for hh in range(2):
                    CP("scalar", h2T32[:, hh * 512:(hh + 1) * 512], ps[3 + hh][:, :], [PS[3 + hh]], ["h2T32"])
                for k in range(8):
                    MM(ps[5][:, 0:72], h2T32[:, k * 128:(k + 1) * 128], v3(wr32, 8, 72)[:, k, :], k == 0, k == 7, ["h2T32", "wr32"], [PS[5]])
                lg = rt[:, 0:72]
                gmax, negm, se, gw = rt[:, 72:73], rt[:, 73:74], rt[:, 74:75], rt[:, 75:76]
                ohg = rt[:, 80:88]
                eg = rt[:, 88:96]
                els = rt[:, 96:104]
                oh1 = rt[:, 104:112]
                els2 = rt[:, 112:120]
                oh2 = rt[:, 120:128]
                m1_, m2_, dm, ed = rt[:, 128:129], rt[:, 129:130], rt[:, 130:131], rt[:, 131:132]
                w1_, w2_ = rt[:, 132:133], rt[:, 133:134]
                wv = rt[:, 136:144]
                t64 = rt[:, 144:208]
                R_ = ["rt"]
                TT("vector", lg, ps[5][:, 0:72], brt, ALU.add, [PS[5], "brt"], R_)
                RED("vector", gmax, rt[:, 0:8], ALU.max, R_, R_)
                TS("vector", ohg, rt[:, 0:8], gmax, ALU.subtract, R_, R_)
                TS("vector", ohg, ohg, 1e30, ALU.mult, R_, R_, s2=1.0, op1=ALU.add)
                TS("vector", ohg, ohg, 0.0, ALU.max, R_, R_, s2=1.0, op1=ALU.min)
                TS("vector", negm, gmax, -1.0, ALU.mult, R_, R_)
                MS("vector", se, 0.0, R_)
                ACT(eg, rt[:, 0:8], AF.Exp, R_, R_, bias=negm, accum=se)
                RCP(gw, se, R_, R_)
                TT("vector", v3(t64, 8, 8), v3(rt[:, 8:72], 8, 8), ohg.unsqueeze(2).to_broadcast([128, 8, 8]), ALU.mult, R_, R_)
                RED("vector", els, v3(t64, 8, 8).rearrange("p g e -> p e g"), ALU.add, R_, R_)
                RED("vector", m1_, els, ALU.max, R_, R_)
                TS("vector", oh1, els, m1_, ALU.subtract, R_, R_)
                TS("vector", oh1, oh1, 1e30, ALU.mult, R_, R_, s2=1.0, op1=ALU.add)
                TS("vector", oh1, oh1, 0.0, ALU.max, R_, R_, s2=1.0, op1=ALU.min)
                STT("vector", els2, oh1, -1e30, els, ALU.mult, ALU.add, R_, R_)
                RED("vector", m2_, els2, ALU.max, R_, R_)
                TS("vector", oh2, els2, m2_, ALU.subtract, R_, R_)
                TS("vector", oh2, oh2, 1e30, ALU.mult, R_, R_, s2=1.0, op1=ALU.add)
                TS("vector", oh2, oh2, 0.0, ALU.max, R_, R_, s2=1.0, op1=ALU.min)
                TT("vector", dm, m2_, m1_, ALU.subtract, R_, R_)
                ACT(ed, dm, AF.Exp, R_, R_)
                TS("vector", m1_, ed, 1.0, ALU.add, R_, R_)
                RCP(w1_, m1_, R_, R_)
                TT("vector", w2_, ed, w1_, ALU.mult, R_, R_)
                TT("vector", w1_, w1_, gw, ALU.mult, R_, R_)
                TT("vector", w2_, w2_, gw, ALU.mult, R_, R_)
                TS("vector", wv, oh1, w1_, ALU.mult, R_, R_)
                STT("vector", wv, oh2, w2_, wv, ALU.mult, ALU.add, R_, R_)
                TT("vector", v3(wg_all[:, tt * 64:(tt + 1) * 64], 8, 8), ohg.unsqueeze(2).to_broadcast([128, 8, 8]),
                   wv.unsqueeze(1).to_broadcast([128, 8, 8]), ALU.mult, R_, ["wg_all"])
            DMA("sync", WGT_d[:, :], wg_all, ["wg_all"], ["WGT_d"])
            A.release(m)
            S.barrier()

        def stageF3():
            A.release(mark_low)
            m = A.mark()
            h2T = A.alloc(8 * NQ, BF16)
            acc = A.alloc(16 * D, F32)
            wgt = A.alloc(16 * 64, F32)
            wex = [A.alloc(12 * 1024, BF16) for _ in range(2)]
            stage = [A.alloc(4 * 512, F32) for _ in range(2)]
            actT = [A.alloc(4 * 512, BF16) for _ in range(2)]
            s1 = [A.alloc(512, F32) for _ in range(2)]
            h3 = v3(h2T, 8, NQ)
            DMA("sync", h3, H2T_d[:, :, :], ["H2T_d0", "H2T_d1"], ["h2Tm"])
            DMA("sync", wgt, WGT_d[:, :], ["WGT_d"], ["wgt"])
            MS("vector", acc, 0.0, ["acc"])
            w1v = w1_e.rearrange("e (k p) n -> e p k n", p=128)
            w3v = w3_e.rearrange("e (k p) n -> e p k n", p=128)
            w2v = w2_e.rearrange("e (k p) n -> e p k n", p=128)
            pc = [0]

            def load_expert(e):
                wb_ = wex[e % 2]
                wk = "wex%d" % (e % 2)
                w1b, w3b, w2b = v3(wb_[:, 0:4096], 8, 512), v3(wb_[:, 4096:8192], 8, 512), v3(wb_[:, 8192:12288], 4, 1024)
                pieces = []
                for hh in range(2):
                    pieces.append((w1b[:, hh * 4:(hh + 1) * 4, :], w1v[e, :, hh * 4:(hh + 1) * 4, :], 4, 512))
                for hh in range(2):
                    pieces.append((w3b[:, hh * 4:(hh + 1) * 4, :], w3v[e, :, hh * 4:(hh + 1) * 4, :], 4, 512))
                for hh in range(2):
                    pieces.append((w2b[:, hh * 2:(hh + 1) * 2, :], w2v[e, :, hh * 2:(hh + 1) * 2, :], 2, 1024))
                for dstv, srcv, a_, b_ in pieces:
                    sb2 = stage[pc[0] % 2]
                    sk = "mstage%d" % (pc[0] % 2)
                    DMA("sync" if pc[0] % 2 == 0 else "scalar", v3(sb2, a_, b_), srcv, [], [sk])
                    CP("gpsimd", dstv, v3(sb2, a_, b_), [sk], [wk])
                    pc[0] += 1

            def wviews(e):
                wb_ = wex[e % 2]
                return v3(wb_[:, 0:4096], 8, 512), v3(wb_[:, 4096:8192], 8, 512), v3(wb_[:, 8192:12288], 4, 1024), "wex%d" % (e % 2)

            def h_phase(it):
                e, tb = divmod(it, 4)
                w1b, w3b, w2b, wk = wviews(e)
                at = actT[it % 2]
                ak = "actT%d" % (it % 2)
                for ft in range(4):
                    pa, pb2 = (0, 1) if ft % 2 == 0 else (2, 3)
                    for k in range(8):
                        MM(ps[pa][:, :], w1b[:, k, ft * 128:(ft + 1) * 128], h3[:, k, tb * 512:(tb + 1) * 512], k == 0, k == 7, [wk, "h2Tm"],
                           [PS[pa]])
                    for k in range(8):
                        MM(ps[pb2][:, :], w3b[:, k, ft * 128:(ft + 1) * 128], h3[:, k, tb * 512:(tb + 1) * 512], k == 0, k == 7, [wk, "h2Tm"],
                           [PS[pb2]])
                    ACT(s1[ft % 2], ps[pa][:, :], AF.Silu, [PS[pa]], ["s1%d" % (ft % 2)])
                    TT("vector", at[:, ft * 512:(ft + 1) * 512], s1[ft % 2], ps[pb2][:, :], ALU.mult, ["s1%d" % (ft % 2), PS[pb2]], [ak])

            def y_phase(it):
                e, tb = divmod(it, 4)
                w1b, w3b, w2b, wk = wviews(e)
                at = actT[it % 2]
                ak = "actT%d" % (it % 2)
                for t4 in range(4):
                    tile_ = tb * 4 + t4
                    for half in range(2):
                        py = 4 + half
                        for ft in range(4):
                            MM(ps[py][:, :], at[:, ft * 512 + t4 * 128:ft * 512 + (t4 + 1) * 128], w2b[:, ft, half * 512:(half + 1) * 512],
                               ft == 0, ft == 3, [ak, wk], [PS[py]])
                        asl = acc[:, tile_ * D + half * 512:tile_ * D + (half + 1) * 512]
                        STT("vector", asl, ps[py][:, :], wgt[:, tile_ * 64 + e:tile_ * 64 + e + 1], asl, ALU.mult, ALU.add,
                            [PS[py], "wgt", "acc"], ["acc"])

            load_expert(0)
            h_phase(0)
            for it in range(256):
                e, tb = divmod(it, 4)
                if tb == 0 and e + 1 < 64:
                    load_expert(e + 1)
                if it + 1 < 256:
                    h_phase(it + 1)
                y_phase(it)
            gfin = A.alloc(D, F32)
            xt = [A.alloc(D, F32) for _ in range(2)]
            junk = A.alloc(D, BF16)
            st4 = A.alloc(8, F32)
            DMA("sync", gfin, g_fin.partition_broadcast(128), [], ["gfin"])
            if "MOE_d" in dbg:
                DMA("sync", MOE_d.rearrange("(t p) d -> p t d", p=128), v3(acc, 16, D), ["acc"], ["MOE_d"])
            for tt in range(16):
                i = tt % 2
                x_ = xt[i]
                xk = "fx%d" % i
                DMA("sync", x_, X1_d[tt * 128:(tt + 1) * 128, :], ["X1_d0", "X1_d1"], [xk])
                asl = acc[:, tt * D:(tt + 1) * D]
                TT("vector", asl, asl, GT2, ALU.mult, ["acc", "GT2"], ["acc"])
                TT("gpsimd", x_, x_, asl, ALU.add, [xk, "acc"], [xk])
                MS("vector", st4[:, 0:1], 0.0, ["st4"])
                ACT(junk, x_, AF.Square, [xk, "st4"], ["fjunk", "st4"], accum=st4[:, 0:1])
                ACT(st4[:, 1:2], st4[:, 0:1], AF.Sqrt, ["st4", "epst"], ["st4"], bias=epst[:, 0:1], scale=1.0 / D)
                RCP(st4[:, 2:3], st4[:, 1:2], ["st4"], ["st4"])
                STT("vector", x_, x_, st4[:, 2:3], gfin, ALU.mult, ALU.mult, [xk, "st4", "gfin"], [xk])
                DMA("sync", out[tt * 128:(tt + 1) * 128, :], x_, [xk], ["out%d" % i])
            A.release(m)

        G1c, SH1c = stageA()
        if upto == "A":
            S.emit(st)
            return nc
        stageB(G1c, SH1c)
        if upto == "B":
            S.emit(st)
            return nc
        if "C1" not in skip:
            stageC1()
        if upto == "C1":
            S.emit(st)
            return nc
        if "D" not in skip:
            stageD()
        if upto == "D":
            S.emit(st)
            return nc
        stageC2()
        if upto == "C2":
            S.emit(st)
            return nc
        stageE()
        if upto == "E":
            S.emit(st)
            return nc
        stageF1()
        stageF2()
        stageF3()
        S.emit(st)
        nc._sched = S
        nc._arena_peak = A.peak
    return nc


def make_in_maps(inputs):
    f = lambda a: np.ascontiguousarray(np.asarray(a, dtype=np.float32))
    x, c, ctx, c_ctx = f(inputs["x"]), f(inputs["c"]), f(inputs["ctx"]), f(inputs["c_ctx"])
    w_in, b_in = f(inputs["w_in"])[0], f(inputs["b_in"])[0]
    wqk, bqk = f(inputs["w_qk_conv"])[0], f(inputs["b_qk_conv"])[0]
    whc, bhc = f(inputs["w_h_conv"])[0], f(inputs["b_h_conv"])[0]
    qkpar = np.stack([wqk[0], wqk[1], wqk[2], bqk, b_in[0:2048]], axis=-1).reshape(16, 128, 5).transpose(1, 0, 2)
    hypar = np.stack([whc[0], whc[1], whc[2], bhc, b_in[HY0:GA0]], axis=-1).reshape(24, 128, 5).transpose(1, 0, 2)
    gpar = np.zeros((36, 2), np.float32)
    gpar[0:4, 0] = b_in[IG0:IG0 + 4]
    gpar[32:36, 0] = b_in[IG0 + 4:IG0 + 8]
    gpar[0:4, 1] = b_in[FG0:FG0 + 4]
    gpar[32:36, 1] = b_in[FG0 + 4:FG0 + 8]
    hfpar = np.zeros((128, 4), np.float32)
    for r in range(2):
        hfpar[r * 64:(r + 1) * 64, 0] = f(inputs["hf_b1"])[0]
        hfpar[r * 64:(r + 1) * 64, 1] = f(inputs["hf_b2"])[0]
        hfpar[r * 64:(r + 1) * 64, 2] = f(inputs["hf_freq"])[0]
    w_rt = np.concatenate([f(inputs["w_group"])[0], f(inputs["w_router"])[0]], axis=1)
    b_rt = np.concatenate([f(inputs["b_group"])[0], f(inputs["b_router"])[0]], axis=0)
    shared = {
        "w_mod": f(inputs["w_mod"])[0], "b_mod": f(inputs["b_mod"]), "g1": f(inputs["g_norm1"])[0], "g2": f(inputs["g_norm2"])[0],
        "w_in": w_in, "b_in": b_in, "qkpar": np.ascontiguousarray(qkpar.reshape(128, 80)),
        "hypar": np.ascontiguousarray(hypar.reshape(128, 120)), "gpar": gpar,
        "hf_w1": f(inputs["hf_w1"])[0], "hfpar": hfpar, "hf_w2": f(inputs["hf_w2"])[0], "hf_w3": f(inputs["hf_w3"])[0],
        "h_bias": f(inputs["h_bias"])[0], "w_a": f(inputs["w_a"])[0], "w_b": f(inputs["w_b"])[0], "w_out": f(inputs["w_out"])[0],
        "w_rt": np.ascontiguousarray(w_rt), "b_rt": np.ascontiguousarray(b_rt),
        "w1_e": f(inputs["w1_e"])[0], "w3_e": f(inputs["w3_e"])[0], "w2_e": f(inputs["w2_e"])[0], "g_fin": f(inputs["g_final"]),
    }
    shared.update(host_consts())
    maps = []
    for i in range(8):
        b, q = i // 4, i % 4
        cT = np.stack([c[b], c_ctx], axis=-1).reshape(8, 128, 2).transpose(1, 0, 2).reshape(128, 16)
        idx = (q * NQ + np.arange(NQ, dtype=np.int32)).reshape(16, 128).T
        m = dict(shared)
        m.update({"xb": x[b], "ctxb": ctx[b], "xq": np.ascontiguousarray(x[b, q * NQ:(q + 1) * NQ]),
                  "idxq": np.ascontiguousarray(idx.astype(np.int32)), "cT": np.ascontiguousarray(cT)})
        maps.append(m)
    return maps


_NC_CACHE = {}


def kernel(**inputs):
    maps = make_in_maps(inputs)
    if "nc" not in _NC_CACHE:
        _NC_CACHE["nc"] = build_nc()
    nc = _NC_CACHE["nc"]
    maps = [{k: m[k] for k in nc._in_names} for m in maps]
    res = run_bass_kernel_spmd(nc, maps, core_ids=list(range(8)))
    outp = np.zeros((2, L, D), np.float32)
    for i in range(8):
        b, q = i // 4, i % 4
        outp[b, q * NQ:(q + 1) * NQ] = np.asarray(res.results[i]["out"], dtype=np.float32)
    return outp
```
